# Optimizing a Trainium2 kernel written in Bass

```python
import jax, jax.numpy as jnp
from jax import lax
import numpy as np

D_MODEL = 1024
BATCH = 4
SEQ = 4096
DEPTH = 2

CHUNK = 64
D_MIX = D_MODEL
LRU_WIDTH = D_MIX // 4
LRU_HEADS = 4
LRU_HEAD_DIM = LRU_WIDTH // LRU_HEADS
CONV_WIDTH = 4
LRU_C = 8.0
FOX_WIDTH = D_MIX // 2
FOX_HEAD_DIM = 64
FOX_HEADS = FOX_WIDTH // FOX_HEAD_DIM
Q_BLOCK = 128
RWKV_WIDTH = D_MIX - LRU_WIDTH - FOX_WIDTH
RWKV_HEAD_DIM = 64
RWKV_HEADS = RWKV_WIDTH // RWKV_HEAD_DIM
DECAY_RANK = 32
AAA_RANK = 32
GATE_RANK = 64
N_GROUPS = 4
EXPERTS_PER_GROUP = 4
N_EXPERTS = N_GROUPS * EXPERTS_PER_GROUP
TOP_K = 2
D_EXPERT = 256
NORM_EPS = 1e-6
GN_EPS = 64e-5
LRU_COLS = 2 * LRU_WIDTH
FOX_COLS = 3 * FOX_WIDTH + FOX_HEADS
RWKV_COLS = 3 * RWKV_WIDTH + DECAY_RANK + AAA_RANK + GATE_RANK
D_IN = LRU_COLS + FOX_COLS + RWKV_COLS

kernel_name = 'hybrid_rglru_fox_rwkv7_hiermoe'


def rms_norm(x, g, eps=NORM_EPS):
    xf = x.astype(jnp.float32)
    y = xf * lax.rsqrt(jnp.mean(xf * xf, axis=-1, keepdims=True) + eps)
    return (y * g.astype(jnp.float32)).astype(x.dtype)


def rglru_group(x_in, gate_in, conv_w, conv_b, wa, ba, wx, bx, lam, norm_g):
    f32 = jnp.float32
    B, S, W = x_in.shape
    xp = jnp.pad(x_in, ((0, 0), (CONV_WIDTH - 1, 0), (0, 0)))
    xc = conv_b + sum(xp[:, j:j + S] * conv_w[j] for j in range(CONV_WIDTH))
    xh = xc.reshape(B, S, LRU_HEADS, LRU_HEAD_DIM)
    r = jax.nn.sigmoid((jnp.einsum('bshi,hij->bshj', xh, wa).reshape(B, S, W) + ba).astype(f32))
    i = jax.nn.sigmoid((jnp.einsum('bshi,hij->bshj', xh, wx).reshape(B, S, W) + bx).astype(f32))
    log_a = -LRU_C * r * jax.nn.softplus(-lam.astype(f32))
    a = jnp.exp(log_a)
    b = jnp.sqrt(-jnp.expm1(2.0 * log_a)) * (i * xc.astype(f32))

    def combine(left, right):
        a1, b1 = left
        a2, b2 = right
        return a1 * a2, a2 * b1 + b2

    _, h = lax.associative_scan(combine, (a, b), axis=1)
    y = jax.nn.gelu(gate_in.astype(f32)) * h
    return rms_norm(y.astype(x_in.dtype), norm_g)


def fox_group(q, k, v, f_logit, fb, qn_g, kn_g, norm_g):
    f32 = jnp.float32
    B, S, _ = q.shape
    H, Dh = FOX_HEADS, FOX_HEAD_DIM
    qh = rms_norm(q.reshape(B, S, H, Dh), qn_g).transpose(0, 2, 1, 3)
    kh = rms_norm(k.reshape(B, S, H, Dh), kn_g).transpose(0, 2, 1, 3)
    vh = v.reshape(B, S, H, Dh).transpose(0, 2, 1, 3)
    log_f = jax.nn.log_sigmoid((f_logit + fb).astype(f32))
    c = jnp.cumsum(log_f, axis=1).transpose(0, 2, 1)
    nb = S // Q_BLOCK
    q_blocks = qh.reshape(B, H, nb, Q_BLOCK, Dh).transpose(2, 0, 1, 3, 4)
    c_blocks = c.reshape(B, H, nb, Q_BLOCK).transpose(2, 0, 1, 3)
    starts = jnp.arange(nb, dtype=jnp.int32) * Q_BLOCK
    k_pos = jnp.arange(S, dtype=jnp.int32)
    scale = FOX_HEAD_DIM ** -0.5

    def block(args):
        qb, cb, s0 = args
        s = jnp.einsum('bhqd,bhkd->bhqk', qb, kh).astype(f32) * scale
        s = s + cb[..., None] - c[:, :, None, :]
        q_pos = s0 + jnp.arange(Q_BLOCK, dtype=jnp.int32)
        mask = k_pos[None, :] <= q_pos[:, None]
        p = jax.nn.softmax(jnp.where(mask, s, -jnp.inf), axis=-1)
        return jnp.einsum('bhqk,bhkd->bhqd', p.astype(vh.dtype), vh)

    o = lax.map(block, (q_blocks, c_blocks, starts))
    o = o.transpose(1, 0, 3, 2, 4).reshape(B, S, FOX_WIDTH)
    return rms_norm(o.astype(q.dtype), norm_g)


def rwkv7_group(p, mu, w0, w2, a0, a2, g2, k_k, k_a, r_k, ln_g, ln_b):
    f32 = jnp.float32
    B, S, _ = p.shape
    H, N = RWKV_HEADS, RWKV_HEAD_DIM
    prev = jnp.pad(p, ((0, 0), (1, 0), (0, 0)))[:, :-1]
    p = p + (prev - p) * mu
    o1 = RWKV_WIDTH
    o2 = 2 * RWKV_WIDTH
    o3 = 3 * RWKV_WIDTH
    o4 = o3 + DECAY_RANK
    o5 = o4 + AAA_RANK
    r, k, v, wd, ad, gd = jnp.split(p, [o1, o2, o3, o4, o5], axis=-1)
    r = r.astype(f32)
    k = k.astype(f32)
    v = v.astype(f32)
    w = -jax.nn.softplus(-(w0 + jnp.tanh(wd) @ w2).astype(f32)) - 0.5
    decay = jnp.exp(-jnp.exp(w))
    a = jax.nn.sigmoid((a0 + ad @ a2).astype(f32))
    g = (jax.nn.sigmoid(gd) @ g2).astype(f32)

    def heads(t):
        return t.reshape(B, S, H, N)

    kk = heads(k * k_k)
    kk = kk / jnp.maximum(jnp.sqrt(jnp.sum(kk * kk, axis=-1, keepdims=True)), 1e-12)
    k = k * (1.0 + (a - 1.0) * k_a)
    rh, wh, kh, vh, ah = heads(r), heads(decay), heads(k), heads(v), heads(a)
    n_chunks = S // CHUNK

    def to_chunks(t):
        return t.transpose(1, 0, 2, 3).reshape(n_chunks, CHUNK, B, H, N)

    xs = (to_chunks(rh), to_chunks(wh), to_chunks(kh), to_chunks(vh), to_chunks(kk), to_chunks(kk * ah))

    def frame_step(state, inp):
        r_t, w_t, k_t, v_t, kk_t, b_t = inp
        sa = jnp.einsum('bhvk,bhk->bhv', state, -kk_t)
        state = (state * w_t[:, :, None, :] + sa[..., None] * b_t[:, :, None, :]
                 + v_t[..., None] * k_t[:, :, None, :])
        return state, jnp.einsum('bhvk,bhk->bhv', state, r_t)

    def chunk_step(state, chunk):
        return lax.scan(frame_step, state, chunk)

    state0 = jnp.zeros((B, H, N, N), f32)
    _, o = lax.scan(chunk_step, state0, xs)
    o = o.reshape(S, B, H, N).transpose(1, 0, 2, 3)
    mean = jnp.mean(o, axis=-1, keepdims=True)
    var = jnp.mean(jnp.square(o - mean), axis=-1, keepdims=True)
    o = ((o - mean) * lax.rsqrt(var + GN_EPS)).reshape(B, S, RWKV_WIDTH) * ln_g + ln_b
    bonus = jnp.sum(rh * kh * r_k, axis=-1, keepdims=True) * vh
    return (o + bonus.reshape(B, S, RWKV_WIDTH)) * g


def hier_moe(x, wg, bg, we, be, w_gate, w_up, w_down):
    f32 = jnp.float32
    B, S, D = x.shape
    t = x.reshape(B * S, D)
    g_probs = jax.nn.softmax((t @ wg).astype(f32) + bg, axis=-1)
    g_p, g_idx = lax.top_k(g_probs, 1)
    e_logits = ((t @ we).astype(f32) + be).reshape(-1, N_GROUPS, EXPERTS_PER_GROUP)
    sel = jax.nn.one_hot(g_idx[:, 0], N_GROUPS, dtype=f32)
    e_logits = jnp.einsum('tg,tge->te', sel, e_logits)
    e_p, e_idx = lax.top_k(jax.nn.softmax(e_logits, axis=-1), TOP_K)
    e_p = e_p / jnp.sum(e_p, axis=-1, keepdims=True)
    weights = g_p * e_p
    ids = g_idx * EXPERTS_PER_GROUP + e_idx
    combine = jnp.sum(jax.nn.one_hot(ids, N_EXPERTS, dtype=f32) * weights[..., None], axis=1)
    hid = jax.nn.silu(jnp.einsum('td,edf->tef', t, w_gate)) * jnp.einsum('td,edf->tef', t, w_up)
    hid = hid * combine[..., None].astype(hid.dtype)
    y = jnp.einsum('tef,efd->td', hid, w_down)
    return y.reshape(B, S, D).astype(x.dtype)


def setup_inputs(seed: int = 0) -> dict:
    key = jax.random.key(seed)
    ks = iter(jax.random.split(key, 40))
    L = DEPTH

    def nrm(shape, scale):
        return jax.random.normal(next(ks), shape, jnp.float32) * scale

    def uni(shape, lo, hi):
        return jax.random.uniform(next(ks), shape, jnp.float32, lo, hi)

    x = nrm((BATCH, SEQ, D_MODEL), 1.0)
    norm1_g = 1.0 + nrm((L, D_MODEL), 0.02)
    w_in = nrm((L, D_MODEL, D_IN), D_MODEL ** -0.5)
    conv_w = nrm((L, CONV_WIDTH, LRU_WIDTH), CONV_WIDTH ** -0.5)
    conv_b = nrm((L, LRU_WIDTH), 0.01)
    lru_wa = nrm((L, LRU_HEADS, LRU_HEAD_DIM, LRU_HEAD_DIM), LRU_HEAD_DIM ** -0.5)
    lru_ba = nrm((L, LRU_WIDTH), 0.01)
    lru_wx = nrm((L, LRU_HEADS, LRU_HEAD_DIM, LRU_HEAD_DIM), LRU_HEAD_DIM ** -0.5)
    lru_bx = nrm((L, LRU_WIDTH), 0.01)
    a_pow = uni((L, LRU_WIDTH), 0.9, 0.999)
    s = a_pow ** (1.0 / LRU_C)
    lru_lambda = jnp.log(s) - jnp.log1p(-s)
    lru_norm_g = 1.0 + nrm((L, LRU_WIDTH), 0.02)
    fox_fb = uni((L, FOX_HEADS), 1.0, 5.0)
    fox_qnorm_g = 1.0 + nrm((L, FOX_HEAD_DIM), 0.02)
    fox_knorm_g = 1.0 + nrm((L, FOX_HEAD_DIM), 0.02)
    fox_norm_g = 1.0 + nrm((L, FOX_WIDTH), 0.02)
    rwkv_mu = uni((L, RWKV_COLS), 0.0, 1.0)
    rwkv_w0 = uni((L, RWKV_WIDTH), -4.0, 0.0)
    rwkv_w2 = nrm((L, DECAY_RANK, RWKV_WIDTH), 0.1 * DECAY_RANK ** -0.5)
    rwkv_a0 = nrm((L, RWKV_WIDTH), 0.1)
    rwkv_a2 = nrm((L, AAA_RANK, RWKV_WIDTH), 0.5 * AAA_RANK ** -0.5)
    rwkv_g2 = nrm((L, GATE_RANK, RWKV_WIDTH), GATE_RANK ** -0.5)
    rwkv_kk = 0.85 + nrm((L, RWKV_WIDTH), 0.05)
    rwkv_ka = 1.0 + nrm((L, RWKV_WIDTH), 0.05)
    rwkv_rk = nrm((L, RWKV_HEADS, RWKV_HEAD_DIM), 0.1)
    rwkv_ln_g = 1.0 + nrm((L, RWKV_WIDTH), 0.02)
    rwkv_ln_b = nrm((L, RWKV_WIDTH), 0.01)
    w_out = nrm((L, D_MIX, D_MODEL), D_MIX ** -0.5)
    norm2_g = 1.0 + nrm((L, D_MODEL), 0.02)
    router_gw = nrm((L, D_MODEL, N_GROUPS), D_MODEL ** -0.5)
    router_gb = nrm((L, N_GROUPS), 0.01)
    router_ew = nrm((L, D_MODEL, N_EXPERTS), D_MODEL ** -0.5)
    router_eb = nrm((L, N_EXPERTS), 0.01)
    exp_w_gate = nrm((L, N_EXPERTS, D_MODEL, D_EXPERT), D_MODEL ** -0.5)
    exp_w_up = nrm((L, N_EXPERTS, D_MODEL, D_EXPERT), D_MODEL ** -0.5)
    exp_w_down = nrm((L, N_EXPERTS, D_EXPERT, D_MODEL), D_EXPERT ** -0.5)
    return {'x': x, 'norm1_g': norm1_g, 'w_in': w_in, 'conv_w': conv_w, 'conv_b': conv_b,
            'lru_wa': lru_wa, 'lru_ba': lru_ba, 'lru_wx': lru_wx, 'lru_bx': lru_bx,
            'lru_lambda': lru_lambda, 'lru_norm_g': lru_norm_g, 'fox_fb': fox_fb,
            'fox_qnorm_g': fox_qnorm_g, 'fox_knorm_g': fox_knorm_g, 'fox_norm_g': fox_norm_g,
            'rwkv_mu': rwkv_mu, 'rwkv_w0': rwkv_w0, 'rwkv_w2': rwkv_w2, 'rwkv_a0': rwkv_a0,
            'rwkv_a2': rwkv_a2, 'rwkv_g2': rwkv_g2, 'rwkv_kk': rwkv_kk, 'rwkv_ka': rwkv_ka,
            'rwkv_rk': rwkv_rk, 'rwkv_ln_g': rwkv_ln_g, 'rwkv_ln_b': rwkv_ln_b, 'w_out': w_out,
            'norm2_g': norm2_g, 'router_gw': router_gw, 'router_gb': router_gb,
            'router_ew': router_ew, 'router_eb': router_eb, 'exp_w_gate': exp_w_gate,
            'exp_w_up': exp_w_up, 'exp_w_down': exp_w_down}


def reference(x, norm1_g, w_in, conv_w, conv_b, lru_wa, lru_ba, lru_wx, lru_bx, lru_lambda,
              lru_norm_g, fox_fb, fox_qnorm_g, fox_knorm_g, fox_norm_g, rwkv_mu, rwkv_w0,
              rwkv_w2, rwkv_a0, rwkv_a2, rwkv_g2, rwkv_kk, rwkv_ka, rwkv_rk, rwkv_ln_g,
              rwkv_ln_b, w_out, norm2_g, router_gw, router_gb, router_ew, router_eb,
              exp_w_gate, exp_w_up, exp_w_down):
    splits = [LRU_WIDTH, LRU_COLS, LRU_COLS + FOX_WIDTH, LRU_COLS + 2 * FOX_WIDTH,
              LRU_COLS + 3 * FOX_WIDTH, LRU_COLS + FOX_COLS]
    for l in range(DEPTH):
        h = rms_norm(x, norm1_g[l])
        proj = h @ w_in[l]
        xa, ga, q, k, v, fl, pc = jnp.split(proj, splits, axis=-1)
        ya = rglru_group(xa, ga, conv_w[l], conv_b[l], lru_wa[l], lru_ba[l], lru_wx[l],
                         lru_bx[l], lru_lambda[l], lru_norm_g[l])
        yb = fox_group(q, k, v, fl, fox_fb[l], fox_qnorm_g[l], fox_knorm_g[l], fox_norm_g[l])
        yc = rwkv7_group(pc, rwkv_mu[l], rwkv_w0[l], rwkv_w2[l], rwkv_a0[l], rwkv_a2[l],
                         rwkv_g2[l], rwkv_kk[l], rwkv_ka[l], rwkv_rk[l], rwkv_ln_g[l], rwkv_ln_b[l])
        y = jnp.concatenate([ya, yb.astype(x.dtype), yc.astype(x.dtype)], axis=-1)
        x = x + (y @ w_out[l]).astype(x.dtype)
        x = x + hier_moe(rms_norm(x, norm2_g[l]), router_gw[l], router_gb[l], router_ew[l],
                         router_eb[l], exp_w_gate[l], exp_w_up[l], exp_w_down[l])
    return x
```

```python
import numpy as np
from contextlib import ExitStack
import concourse.bass as bass
import concourse.mybir as mybir
from concourse.bass_utils import run_bass_kernel_spmd

F32 = mybir.dt.float32
BF16 = mybir.dt.bfloat16
AF = mybir.ActivationFunctionType
ALU = mybir.AluOpType
AX = mybir.AxisListType
ENGS = ("pe", "act", "dve", "pool", "sp")

D = 1024
NE = 16
DIN = 2952
NORM_EPS = 1e-6
GN_EPS = 64e-5
import os
PASSES = os.environ.get('KPASSES', 'PABCOM')
STQ = os.environ.get('KSTQ', 'sp')


class T:
    def __init__(s, ap, key):
        s.ap = ap
        s.key = key

    def __getitem__(s, idx):
        return T(s.ap[idx], s.key)

    def re(s, pat, **kw):
        return T(s.ap.rearrange(pat, **kw), s.key)

    def bc(s, axis, shape):
        return T(s.ap.unsqueeze(axis).broadcast_to(shape), s.key)


def U(x):
    return x.ap if isinstance(x, T) else x


def KEYS(*xs):
    out = []
    for x in xs:
        if isinstance(x, T):
            if isinstance(x.key, tuple):
                out.extend(x.key)
            else:
                out.append(x.key)
    return out


class _Ins:
    __slots__ = ("eng", "fn", "deps", "dma", "sig", "val", "sem", "idx", "flushed", "tail")


class Prog:
    ND = 48

    def __init__(self, nc, stack):
        self.nc = nc
        self.stack = stack
        self.q = {e: [] for e in ENGS}
        self.lastw = {}
        self.readers = {}
        self.ndma = 0
        self.dma_ins = []
        self.nt = 0
        self.csem = {e: stack.enter_context(nc.semaphore(f"c_{e}")) for e in ENGS}
        self.dsem = [stack.enter_context(nc.semaphore(f"d_{i}")) for i in range(self.ND)]
        self.cnt = {e: 0 for e in ENGS}
        self.seen = {e: {} for e in ENGS}
        self.pending = {e: [] for e in ENGS}
        self.last_flushed = {e: None for e in ENGS}
        self.ph = None
        self.ninstr = 0
        self.cut = None

    def phase(self):
        self.ph = ExitStack()
        return self.ph

    def sb(self, shape, dt=F32, name=None):
        self.nt += 1
        nm = name or f"t{self.nt}"
        t = self.ph.enter_context(self.nc.sbuf_tensor(f"{nm}_{self.nt}", list(shape), dt))
        return T(t[:] if not hasattr(t, "ap") or True else t, nm + str(self.nt))

    def ps(self, shape, dt=F32, name=None):
        self.nt += 1
        nm = name or f"p{self.nt}"
        t = self.ph.enter_context(self.nc.psum_tensor(f"{nm}_{self.nt}", list(shape), dt))
        return T(t[:], nm + str(self.nt))

    def _res(self, d):
        if d.flushed and not d.sig:
            return d.tail
        return d

    def op(self, eng, fn, r=(), w=(), dma=False):
        if self.cut is not None:
            self.cut -= 1
            if self.cut < 0:
                return None
            if 'KLIST' in os.environ:
                import sys as _s
                f = _s._getframe(2)
                print('OP', self.cut, eng, f.f_lineno, f.f_locals.get('h'), f.f_locals.get('ct'))
        ins = _Ins()
        ins.eng, ins.fn, ins.dma, ins.sig = eng, fn, dma, dma
        ins.val, ins.sem, ins.flushed, ins.tail = 0, None, False, None
        deps = list(self.pending[eng])
        self.pending[eng] = []
        for k in r:
            lw = self.lastw.get(k)
            if lw is not None:
                deps.append(lw)
        for k in w:
            lw = self.lastw.get(k)
            if lw is not None:
                deps.append(lw)
            deps.extend(self.readers.get(k, ()))
        if dma:
            ins.idx = self.ndma
            if self.ndma >= self.ND:
                deps.append(self.dma_ins[self.ndma - self.ND])
            self.dma_ins.append(ins)
            self.ndma += 1
        dd, seen = [], set()
        for d in deps:
            d = self._res(d)
            if d is None or d is ins or id(d) in seen:
                continue
            if eng == "pe" and d.eng == "pe" and not d.dma:
                continue
            seen.add(id(d))
            dd.append(d)
            d.sig = True
        ins.deps = dd
        for k in r:
            self.readers.setdefault(k, []).append(ins)
        for k in w:
            self.lastw[k] = ins
            self.readers[k] = []
        self.q[eng].append(ins)
        return ins

    def barrier(self):
        deps = [self.last_flushed[e] for e in ENGS if self.last_flushed[e] is not None]
        deps += self.dma_ins[-self.ND:]
        for e in ENGS:
            self.pending[e] = list(deps)

    def flush(self):
        nc = self.nc
        for e in ENGS:
            if self.q[e]:
                tail = None
                for ins in reversed(self.q[e]):
                    if not ins.dma:
                        tail = ins
                        break
                if tail is not None:
                    tail.sig = True
                for ins in self.q[e]:
                    ins.tail = tail
                    if ins.dma:
                        ins.sem = self.dsem[ins.idx % self.ND]
                        ins.val = 16 * (ins.idx // self.ND + 1)
                    elif ins.sig:
                        self.cnt[e] += 1
                        ins.sem = self.csem[e]
                        ins.val = self.cnt[e]
        with nc.Block() as block:
            def run(ename, eobj):
                seen = self.seen[ename]
                for ins in self.q[ename]:
                    for d in ins.deps:
                        key = id(d.sem)
                        if seen.get(key, 0) < d.val:
                            eobj.wait_ge(d.sem, d.val)
                            seen[key] = d.val
                    i = ins.fn(eobj)
                    self.ninstr += 1
                    if ins.dma:
                        i.then_inc(ins.sem, 16)
                    elif ins.sig:
                        i.then_inc(ins.sem, 1)
                for ins in self.q[ename]:
                    if ins.dma:
                        key = id(ins.sem)
                        if seen.get(key, 0) < ins.val:
                            eobj.wait_ge(ins.sem, ins.val)
                            seen[key] = ins.val

            @block.tensor
            def _(e):
                run("pe", e)

            @block.scalar
            def _(e):
                run("act", e)

            @block.vector
            def _(e):
                run("dve", e)

            @block.gpsimd
            def _(e):
                run("pool", e)

            @block.sync
            def _(e):
                run("sp", e)
        for e in ENGS:
            for ins in self.q[e]:
                ins.flushed = True
            if self.q[e]:
                t = self.q[e][-1].tail
                if t is not None:
                    self.last_flushed[e] = t
            self.q[e] = []

    def end_phase(self):
        print('phase sbuf remaining', self.nc.sbuf_bytes_remaining)
        self.flush()
        self.ph.close()
        self.ph = None
        self.barrier()

    def mm(self, out, lhsT, rhs, start=True, stop=True, skip=False):
        self.op("pe", lambda e: e.matmul(out=U(out), lhsT=U(lhsT), rhs=U(rhs), start=start, stop=stop,
                                         skip_group_check=skip),
                r=KEYS(lhsT, rhs), w=KEYS(out))

    def tr(self, out, in_, ident):
        self.op("pe", lambda e: e.transpose(out=U(out), in_=U(in_), identity=U(ident)),
                r=KEYS(in_, ident), w=KEYS(out))

    def act(self, out, in_, func, bias=None, scale=None, accum=None):
        kw = {}
        if bias is not None:
            kw["bias"] = U(bias)
        if scale is not None:
            kw["scale"] = U(scale)
        if accum is not None:
            kw["accum_out"] = U(accum)
        self.op("act", lambda e: e.activation(out=U(out), in_=U(in_), func=func, **kw),
                r=KEYS(in_, bias, scale), w=KEYS(out, accum))

    def ts(self, eng, out, in0, s1, op0, s2=None, op1=None):
        kw = {}
        if op1 is not None:
            kw["op1"] = op1
        self.op(eng, lambda e: e.tensor_scalar(out=U(out), in0=U(in0), scalar1=U(s1), scalar2=U(s2), op0=op0, **kw),
                r=KEYS(in0, s1, s2), w=KEYS(out))

    def tt(self, eng, out, in0, in1, op):
        self.op(eng, lambda e: e.tensor_tensor(out=U(out), in0=U(in0), in1=U(in1), op=op),
                r=KEYS(in0, in1), w=KEYS(out))

    def stt(self, out, in0, sc, in1, op0, op1):
        self.op("dve", lambda e: e.scalar_tensor_tensor(out=U(out), in0=U(in0), scalar=U(sc), in1=U(in1), op0=op0, op1=op1),
                r=KEYS(in0, sc, in1), w=KEYS(out))

    def cp(self, eng, out, in_):
        if eng == "act":
            self.op("act", lambda e: e.copy(out=U(out), in_=U(in_)), r=KEYS(in_), w=KEYS(out))
        else:
            self.op(eng, lambda e: e.tensor_copy(out=U(out), in_=U(in_)), r=KEYS(in_), w=KEYS(out))

    def rcp(self, out, in_):
        self.op("dve", lambda e: e.reciprocal(out=U(out), in_=U(in_)), r=KEYS(in_), w=KEYS(out))

    def scan(self, out, d0, d1, init):
        self.op("dve", lambda e: e.tensor_tensor_scan(out=U(out), data0=U(d0), data1=U(d1), initial=U(init),
                                                      op0=ALU.mult, op1=ALU.add),
                r=KEYS(d0, d1, init), w=KEYS(out))

    def red(self, out, in_, op):
        self.op("dve", lambda e: e.tensor_reduce(out=U(out), in_=U(in_), axis=AX.X, op=op), r=KEYS(in_), w=KEYS(out))

    def dma(self, eng, out, in_, **kw):
        self.op(eng, lambda e: e.dma_start(out=U(out), in_=U(in_), **kw), r=KEYS(in_), w=KEYS(out), dma=True)


CST = {"ident": 0, "triI": 128, "MTs": 256, "MTi": 384, "Ms": 512, "bones": 640, "ones": 768, "aug": 896}
NCST = 904


def make_consts():
    c = np.zeros((128, NCST), np.float32)
    p = np.arange(128)
    c[:, 0:128] = np.eye(128)
    c[:, 128:256] = (p[None, :] >= p[:, None])
    same = (p[:, None] // 64) == (p[None, :] // 64)
    mts = same & (p[None, :] > p[:, None])
    mti = same & (p[None, :] >= p[:, None])
    c[:, 256:384] = mts
    c[:, 384:512] = mti
    c[:, 512:640] = mts.T
    c[:, 640:768] = same
    c[:, 768:896] = 1.0
    r = p % 32
    c[:, 896] = -1.0 * ((r == 1) | (r == 2))
    c[:, 897] = -1.0 * (r == 2)
    c[:, 898] = -1.0 * (r < 3)
    c[:, 899] = NORM_EPS
    c[:, 900] = GN_EPS
    c[:, 901] = 1.0
    c[:, 902] = 1e-24
    return c


PVN = {}


def _pv_layout():
    names = []
    names += [f"n1g{c}" for c in range(8)] + [f"n2g{c}" for c in range(8)]
    for ct in range(2):
        names += [f"cw{j}_{ct}" for j in range(4)] + [f"cb_{ct}", f"ba_{ct}", f"bx_{ct}", f"lam_{ct}", f"ng_{ct}"]
    names += ["gq", "gk"] + [f"fngh{h}" for h in range(8)] + [f"fb{a}" for a in range(3)]
    names += [f"mu{i}" for i in range(7)]
    for ct in range(2):
        names += [f"w0_{ct}", f"a0_{ct}", f"kk_{ct}", f"ka_{ct}", f"rk_{ct}", f"lng_{ct}", f"lnb_{ct}"]
    for i, n in enumerate(names):
        PVN[n] = i
    return len(names)


NPV = _pv_layout()


def aug_head(a, i):
    h = 3 * a + i
    return h if h < 8 else None


def make_pv(inp, l):
    pv = np.zeros((128, NPV), np.float32)
    p = np.arange(128)
    for c in range(8):
        pv[:, PVN[f"n1g{c}"]] = inp["norm1_g"][l, c * 128:(c + 1) * 128]
        pv[:, PVN[f"n2g{c}"]] = inp["norm2_g"][l, c * 128:(c + 1) * 128]
    for ct in range(2):
        sl = slice(ct * 128, (ct + 1) * 128)
        for j in range(4):
            pv[:, PVN[f"cw{j}_{ct}"]] = inp["conv_w"][l, j, sl]
        pv[:, PVN[f"cb_{ct}"]] = inp["conv_b"][l, sl]
        pv[:, PVN[f"ba_{ct}"]] = inp["lru_ba"][l, sl]
        pv[:, PVN[f"bx_{ct}"]] = inp["lru_bx"][l, sl]
        pv[:, PVN[f"lam_{ct}"]] = inp["lru_lambda"][l, sl]
        pv[:, PVN[f"ng_{ct}"]] = inp["lru_norm_g"][l, sl]
        pv[:, PVN[f"w0_{ct}"]] = inp["rwkv_w0"][l, sl]
        pv[:, PVN[f"a0_{ct}"]] = inp["rwkv_a0"][l, sl]
        pv[:, PVN[f"kk_{ct}"]] = inp["rwkv_kk"][l, sl]
        pv[:, PVN[f"ka_{ct}"]] = inp["rwkv_ka"][l, sl]
        pv[:, PVN[f"rk_{ct}"]] = inp["rwkv_rk"][l].reshape(256)[sl]
        pv[:, PVN[f"lng_{ct}"]] = inp["rwkv_ln_g"][l, sl]
        pv[:, PVN[f"lnb_{ct}"]] = inp["rwkv_ln_b"][l, sl]
    pv[:, PVN["gq"]] = inp["fox_qnorm_g"][l][p % 64]
    pv[:, PVN["gk"]] = inp["fox_knorm_g"][l][p % 64]
    for h in range(8):
        pv[0:64, PVN[f"fngh{h}"]] = inp["fox_norm_g"][l, h * 64:(h + 1) * 64]
    for a in range(3):
        for i in range(3):
            h = aug_head(a, i)
            if h is not None:
                pv[32 * i:32 * i + 32, PVN[f"fb{a}"]] = inp["fox_fb"][l, h]
    for i in range(7):
        pv[:, PVN[f"mu{i}"]] = inp["rwkv_mu"][l, i * 128:(i + 1) * 128]
    return pv


def make_wflrep(inp, l):
    w = np.zeros((1024, 288), np.float32)
    for a in range(3):
        for i in range(3):
            h = aug_head(a, i)
            if h is not None:
                for rr in range(3):
                    w[:, a * 96 + 32 * i + rr] = inp["w_in"][l, :, 2048 + h]
    return w


def make_lruw(inp, l):
    m = np.zeros((2, 2, 128, 128), np.float32)
    for ct in range(2):
        for k, nm in enumerate(("lru_wa", "lru_wx")):
            for hh in range(2):
                m[ct, k, hh * 64:(hh + 1) * 64, hh * 64:(hh + 1) * 64] = inp[nm][l, ct * 2 + hh]
    return m


PT_TILES = [(128 * t, 128) for t in range(16)] + [(2048 + 96 * a, 96) for a in range(3)] + \
           [(2336 + 128 * i, 128) for i in range(7)]
NPT = len(PT_TILES)
WINB = 3232


def build_program(S, L, dbg=False):
    nc = bass.Bass("TRN2", target_bir_lowering=False)
    NB = S // 512
    NT = S // 128

    def din(name, shape, dt=F32):
        return T(nc.dram_tensor(name, list(shape), dt, kind="ExternalInput").ap(), name)

    def dscr(name, shape, dt=F32):
        kind = "ExternalOutput" if dbg else "Internal"
        return T(nc.dram_tensor(name, list(shape), dt, kind=kind).ap(), name)

    x_in = din("x", [S, D])
    w_in = din("w_in", [L, D, DIN])
    wfl = din("wflrep", [L, D, 288])
    w_out = din("w_out", [L, D, D])
    pvd = din("pv", [L, 128, NPV])
    n1g_d = din("norm1_g", [L, D])
    n2g_d = din("norm2_g", [L, D])
    cstd = din("cst", [128, NCST])
    lruw = din("lruw", [L, 2, 2, 128, 128])
    lrw = din("lrw", [L, 128, 256])
    wr = din("wr", [L, D, 20])
    rb = din("rb", [L, 20])
    wg = din("exp_w_gate", [L, NE, D, 256])
    wu = din("exp_w_up", [L, NE, D, 256])
    wd = din("exp_w_down", [L, NE, 256, D])
    out = T(nc.dram_tensor("out", [S, D], F32, kind="ExternalOutput").ap(), "out")
    projT = dscr("projT", [NPT, 128, S])
    yTd = dscr("yTd", [8, 128, S], BF16)
    xmid = dscr("xmid", [S, D])
    xl = dscr("xl", [S, D])

    with ExitStack() as st:
        P = Prog(nc, st)

        def consts():
            cf = P.sb([128, NCST], F32, "cf")
            cb = P.sb([128, 896], BF16, "cb")
            P.dma("sp", cf, cstd)
            P.cp("dve", cb, cf[:, 0:896])
            return cf, cb

        def banks(n=7):
            return [P.ps([128, 512], F32, f"pb{i}") for i in range(n)], P.ps([128, 1024], BF16, "pbt")

        for l in range(L):
            xsrc = x_in if l == 0 else xl
            xdst = out if l == L - 1 else xl

            def pvc(pv, name):
                return pv[:, PVN[name]:PVN[name] + 1]

            with P.phase():
                cf, cb = consts()
                pb, pbt = banks(3)
                pv = P.sb([128, NPV], F32, "pv")
                P.dma("sp", pv, pvd[l])
                winb = P.sb([128, 8, WINB], BF16, "winb")
                w3 = w_in[l].re("(c p) n -> p c n", p=128)
                for c in range(8):
                    P.dma("pool", winb[:, c, 0:2048], w3[:, c, 0:2048])
                P.dma("pool", winb[:, :, 2336:3232], w3[:, :, 2056:2952])
                P.dma("pool", winb[:, :, 2048:2336], wfl[l].re("(c p) n -> p c n", p=128))
                gbc = P.sb([128, D], F32, "gbc")
                P.dma("sp", gbc, T(U(n1g_d[l]).partition_broadcast(128), n1g_d.key))
                xb = P.sb([128, 4, D], F32, "xb")
                junk = P.sb([128, D], F32, "junk")
                hb = P.sb([128, D], BF16, "hb")
                hT2 = [P.sb([128, 8, 512], BF16, "hT") for _ in range(2)]
                st4 = P.sb([128, 8], F32, "st4")
                ost = [P.sb([128, 512], F32, f"ost{i}") for i in range(3)]
                def normP(blk, par):
                    hT = hT2[par]
                    P.dma("sp", xb, xsrc[blk * 512:(blk + 1) * 512, :].re("(i p) d -> p i d", p=128))
                    for i in range(4):
                        ss = st4[:, i:i + 1]
                        rs = st4[:, 4 + i:5 + i]
                        P.act(junk, xb[:, i, :], AF.Square, accum=ss)
                        P.ts("dve", rs, ss, 1.0 / D, ALU.mult, NORM_EPS, ALU.add)
                        P.act(rs, rs, AF.Sqrt)
                        P.rcp(rs, rs)
                        P.stt(hb, xb[:, i, :], rs, gbc, ALU.mult, ALU.mult)
                        for c in range(8):
                            P.tr(pbt[:, c * 128:(c + 1) * 128], hb[:, c * 128:(c + 1) * 128], cb[:, 0:128])
                        P.cp("act", hT[:, :, i * 128:(i + 1) * 128], pbt.re("p (c t) -> p c t", c=8))
                        yield

                def projP(blk, par):
                    hT = hT2[par]
                    for ot, (co, wdt) in enumerate(PT_TILES):
                        bk = pb[ot % 3]
                        for c in range(8):
                            P.mm(bk[0:wdt, :], winb[:, c, co:co + wdt], hT[:, c, :], start=(c == 0), stop=(c == 7))
                        o_ = ost[ot % 3]
                        P.cp("act" if ot % 2 == 0 else "dve", o_[0:wdt, :], bk[0:wdt, :])
                        P.dma(STQ, projT[ot, 0:wdt, blk * 512:(blk + 1) * 512], o_[0:wdt, :])
                        if ot % 6 == 5:
                            yield

                def driveP(main, side):
                    ms, ss = True, side is not None
                    while ms or ss:
                        if ms:
                            try:
                                next(main)
                            except StopIteration:
                                ms = False
                        if ss:
                            try:
                                next(side)
                            except StopIteration:
                                ss = False

                NBp = NB if 'P' in PASSES else 0
                if NBp:
                    driveP(normP(0, 0), None)
                for blk in range(NBp):
                    driveP(projP(blk, blk % 2), normP(blk + 1, (blk + 1) % 2) if blk + 1 < NBp else None)
                P.end_phase()

            with P.phase():
                cf, cb = consts()
                pb, pbt = banks(5)
                pv = P.sb([128, NPV], F32, "pv")
                P.dma("sp", pv, pvd[l])
                lw = P.sb([128, 4, 128], F32, "lw")
                lwb = P.sb([128, 4, 128], BF16, "lwb")
                P.dma("sp", lw, lruw[l].re("a b p n -> p (a b) n"))
                P.cp("dve", lwb, lw)
                cvec = P.sb([128, 2], F32, "cvec")
                for ct in range(2):
                    P.act(cvec[:, ct:ct + 1], pvc(pv, f"lam_{ct}"), AF.Exp, scale=-1.0)
                    P.act(cvec[:, ct:ct + 1], cvec[:, ct:ct + 1], AF.Ln, bias=cf[:, 901:902])
                    P.ts("dve", cvec[:, ct:ct + 1], cvec[:, ct:ct + 1], -8.0, ALU.mult)
                xaw = [P.sb([128, 515], F32, f"xaw{ct}") for ct in range(2)]
                hc = [P.sb([128, 1], F32, f"hc{ct}") for ct in range(2)]
                for ct in range(2):
                    P.op("pool", lambda e, t=xaw[ct]: e.memset(U(t), 0.0), w=KEYS(xaw[ct]))
                    P.op("pool", lambda e, t=hc[ct]: e.memset(U(t), 0.0), w=KEYS(hc[ct]))
                gin = [P.sb([128, 512], F32, f"gin{ct}") for ct in range(2)]
                sA2 = [[P.sb([128, 512], F32, f"sA{ct}_{i}") for i in range(6)] for ct in range(2)]
                xcb2 = [P.sb([128, 512], BF16, f"xcb{ct}") for ct in range(2)]
                yv = [P.sb([128, 512], F32, f"yv{ct}") for ct in range(2)]
                ysq2 = [P.sb([128, 512], BF16, f"ysq{ct}") for ct in range(2)]
                yo = P.sb([128, 2, 512], BF16, "yo")

                def chainA(blk, ct):
                    cs = slice(blk * 512, (blk + 1) * 512)
                    xcb, ysq = xcb2[ct], ysq2[ct]
                    pra, pia = pb[2 * ct], pb[2 * ct + 1]
                    P.dma("sp", xaw[ct][:, 3:515], projT[ct, :, cs])
                    P.dma("sp", gin[ct], projT[2 + ct, :, cs])
                    xw = xaw[ct]
                    xc, r_, i_, a_, t1, t2 = sA2[ct]
                    P.ts("dve", xc, xw[:, 0:512], pvc(pv, f"cw0_{ct}"), ALU.mult, pvc(pv, f"cb_{ct}"), ALU.add)
                    for j in range(1, 4):
                        P.stt(xc, xw[:, j:j + 512], pvc(pv, f"cw{j}_{ct}"), xc, ALU.mult, ALU.add)
                    yield
                    P.cp("dve", t1[:, 0:3], xw[:, 512:515])
                    P.cp("dve", xw[:, 0:3], t1[:, 0:3])
                    P.cp("act", xcb, xc)
                    P.mm(pra, lwb[:, ct * 2 + 0, :], xcb)
                    P.mm(pia, lwb[:, ct * 2 + 1, :], xcb)
                    yield
                    P.act(r_, pra, AF.Sigmoid, bias=pvc(pv, f"ba_{ct}"))
                    P.act(i_, pia, AF.Sigmoid, bias=pvc(pv, f"bx_{ct}"))
                    yield
                    P.act(a_, r_, AF.Exp, scale=cvec[:, ct:ct + 1])
                    P.act(t1, a_, AF.Square)
                    yield
                    P.ts("dve", t1, t1, -1.0, ALU.mult, 1.0, ALU.add)
                    P.act(t1, t1, AF.Sqrt)
                    P.tt("dve", t2, i_, xc, ALU.mult)
                    yield
                    P.tt("dve", t2, t2, t1, ALU.mult)
                    P.scan(r_, a_, t2, hc[ct])
                    P.cp("dve", hc[ct], r_[:, 511:512])
                    yield
                    P.act(i_, gin[ct], AF.Gelu_apprx_tanh)
                    P.tt("dve", yv[ct], i_, r_, ALU.mult)
                    P.act(ysq, yv[ct], AF.Square)
                    yield

                for blk in range(NB if 'A' in PASSES else 0):
                    cs = slice(blk * 512, (blk + 1) * 512)
                    gens = [chainA(blk, 0), chainA(blk, 1)]
                    while gens:
                        for g_ in list(gens):
                            try:
                                next(g_)
                            except StopIteration:
                                gens.remove(g_)
                    for ct in range(2):
                        P.mm(pb[4], cb[:, 768:896], ysq2[ct], start=(ct == 0), stop=(ct == 1))
                    sd = sA2[0][0]
                    P.act(sd, pb[4], AF.Ln, bias=cf[:, 899:900], scale=1.0 / 256)
                    P.act(sd, sd, AF.Exp, scale=-0.5)
                    for ct in range(2):
                        P.stt(yo[:, ct, :], yv[ct], pvc(pv, f"ng_{ct}"), sd, ALU.mult, ALU.mult)
                    P.dma(STQ, yTd[0:2, :, cs].re("c p t -> p c t"), yo)
                P.end_phase()

            with P.phase():
                cf, cb = consts()
                pb, pbt = banks(7)
                pv = P.sb([128, NPV], F32, "pv")
                P.dma("sp", pv, pvd[l])
                gqs = P.sb([128, 1], F32, "gqs")
                P.ts("dve", gqs, pvc(pv, "gq"), 0.125, ALU.mult)
                nfb = P.sb([128, 3], F32, "nfb")
                P.ts("dve", nfb, pv[:, PVN["fb0"]:PVN["fb0"] + 3], -1.0, ALU.mult)
                kT = P.sb([128, 4, S], BF16, "kT")
                Vp = P.sb([128, NT, 8, 65], BF16, "Vp")
                P.op("pool", lambda e: e.memset(U(Vp), 1.0), w=[f"Vp{b}" for b in range(NB)])
                cpT = P.sb([128, NT, 9], F32, "cpT")
                cpc = P.sb([128, 3], F32, "cpc")
                P.op("pool", lambda e: e.memset(U(cpc), 0.0), w=KEYS(cpc))
                qT2 = [P.sb([128, 4, 512], BF16, "qT") for _ in range(2)]
                qaug2 = [P.sb([128, 3, 512], BF16, "qaug") for _ in range(2)]
                fsq = P.sb([128, 512], BF16, "fsq")
                fsd = P.sb([128, 512], F32, "fsd")

                def KB(t, nm, b):
                    return T(t.ap, f"{nm}{b}")
                ld_ = [P.sb([128, 512], F32, f"ldB{i}") for i in range(3)]
                s = [P.sb([128, 512], F32, f"sB{i}") for i in range(5)]
                sqb = P.sb([128, 512], BF16, "sqb")
                vb = P.sb([128, 512], BF16, "vb")
                pex = [P.sb([128, 512], BF16, f"pex{i}") for i in range(6)]
                onb = P.sb([128, 8, 512], F32, "onb")
                rl = P.sb([128, 512], F32, "rl")
                bcs = P.sb([128, 512], F32, "bcs")
                ybo = P.sb([128, 8, 512], BF16, "ybo")
                ones3 = cb[:, 768:896]
                onesf = P.sb([128, 512], F32, "onesf")
                P.op("pool", lambda e: e.memset(U(onesf), 1.0), w=KEYS(onesf))
                npx = 0
                def preB(blk, par):
                    qT, qaug = qT2[par], qaug2[par]
                    cs = slice(blk * 512, (blk + 1) * 512)
                    for which in range(2):
                        for pp in range(4):
                            t_in = ld_[(which * 4 + pp) % 3]
                            P.dma("sp", t_in, projT[4 + which * 4 + pp, :, cs])
                            P.act(sqb, t_in, AF.Square)
                            P.mm(pb[6], cb[:, 640:768], sqb)
                            sd, r1 = s[0], s[1]
                            P.act(sd, pb[6], AF.Ln, bias=cf[:, 899:900], scale=1.0 / 64)
                            P.act(r1, sd, AF.Exp, scale=-0.5)
                            if which == 0:
                                P.stt(qT[:, pp, :], t_in, gqs, r1, ALU.mult, ALU.mult)
                            else:
                                P.stt(KB(kT, "kT", blk)[:, pp, cs], t_in, pvc(pv, "gk"), r1, ALU.mult, ALU.mult)
                            yield
                    for pp in range(4):
                        t_in = ld_[pp % 3]
                        P.dma("sp", t_in, projT[12 + pp, :, cs])
                        P.cp("act", vb, t_in)
                        for i in range(4):
                            P.tr(pbt[:, i * 128:(i + 1) * 128], vb[:, i * 128:(i + 1) * 128], cb[:, 0:128])
                        P.cp("act", KB(Vp, "Vp", blk)[:, blk * 4:blk * 4 + 4, 2 * pp:2 * pp + 2, 0:64],
                             pbt[:, 0:512].re("p (i h d) -> p i h d", i=4, h=2))
                        yield
                    for a in range(3):
                        t_in = ld_[a % 3]
                        P.dma("sp", t_in, projT[16 + a, :, cs])
                        e_, cp_, hi, t1, t2 = s
                        hib = sqb
                        P.act(e_[0:96], t_in[0:96], AF.Exp, bias=nfb[0:96, a:a + 1], scale=-1.0)
                        P.act(e_[0:96], e_[0:96], AF.Ln, bias=cf[0:96, 901:902])
                        P.scan(cp_[0:96], onesf[0:96], e_[0:96], cpc[0:96, a:a + 1])
                        P.cp("dve", cpc[0:96, a:a + 1], cp_[0:96, 511:512])
                        for i in range(4):
                            P.tr(pb[6][:, i * 96:(i + 1) * 96], cp_[0:96, i * 128:(i + 1) * 128], cf[0:96, 0:96])
                        nh = 3 if a < 2 else 2
                        P.cp("act", KB(cpT, "cpT", blk)[:, blk * 4:blk * 4 + 4, 3 * a:3 * a + nh],
                             pb[6][:, 0:384].re("p (i h r) -> p i h r", i=4, h=3)[:, :, 0:nh, 0])
                        P.cp("dve", hib[0:96], cp_[0:96])
                        P.stt(t1[0:96], hib[0:96], cf[0:96, 896:897], cp_[0:96], ALU.mult, ALU.add)
                        P.cp("dve", hib[0:96], t1[0:96])
                        P.stt(t2[0:96], hib[0:96], cf[0:96, 897:898], t1[0:96], ALU.mult, ALU.add)
                        P.ts("dve", qaug[0:96, a, :], t2[0:96], cf[0:96, 898:899], ALU.mult)
                        yield

                def attB(blk, par):
                    qT, qaug = qT2[par], qaug2[par]
                    cs = slice(blk * 512, (blk + 1) * 512)
                    njt = 4 * blk + 4
                    units = [(pp, jt) for pp in range(4) for jt in range(njt)]

                    def att_s1(u, k):
                        pp, jt = u
                        idg = jt - 4 * blk
                        c0 = 0 if idg < 0 else 128 * idg
                        for hh in range(2):
                            h, hb_ = 2 * pp + hh, hh * 64
                            psb = pb[2 + 2 * (k % 2) + hh]
                            P.mm(psb[:, c0:512], KB(kT, "kT", jt // 4)[hb_:hb_ + 64, pp, jt * 128:(jt + 1) * 128], qT[hb_:hb_ + 64, pp, c0:512],
                                 start=True, stop=False)
                        for hh in range(2):
                            h = 2 * pp + hh
                            a, ab = h // 3, 32 * (h % 3)
                            psb = pb[2 + 2 * (k % 2) + hh]
                            P.mm(psb[:, c0:512], ones3[ab:ab + 3, 0:128], qaug[ab:ab + 3, a, c0:512], start=False, stop=True)
                        for hh in range(2):
                            h = 2 * pp + hh
                            psb, px = pb[2 + 2 * (k % 2) + hh], pex[(k % 3) * 2 + hh]
                            P.act(px[:, c0:512], psb[:, c0:512], AF.Exp, bias=KB(cpT, "cpT", jt // 4)[:, jt, h:h + 1])
                            if idg >= 0:
                                P.tt("dve", px[:, c0:c0 + 128], px[:, c0:c0 + 128], cb[:, 128:256], ALU.mult)

                    def att_s2(u, k):
                        pp, jt = u
                        idg = jt - 4 * blk
                        c0 = 0 if idg < 0 else 128 * idg
                        for hh in range(2):
                            h = 2 * pp + hh
                            po, px = pb[hh], pex[(k % 3) * 2 + hh]
                            P.mm(po[0:65, c0:512], KB(Vp, "Vp", jt // 4)[:, jt, h, :], px[:, c0:512], start=(jt == 0), stop=(jt == njt - 1))
                        if jt == njt - 1:
                            for hh in range(2):
                                h = 2 * pp + hh
                                po, pl = pb[hh], pb[6]
                                P.act(rl[64:65, :], po[64:65, :], AF.Ln)
                                P.mm(pl, cf[64:65, 768:896], rl[64:65, :])
                                P.act(bcs[0:64, :], pl[0:64, :], AF.Exp, scale=-1.0)
                                P.tt("dve", onb[0:64, h, :], po[0:64, :], bcs[0:64, :], ALU.mult)

                    for k in range(len(units) + 1):
                        if k < len(units):
                            att_s1(units[k], k)
                        if k >= 1:
                            att_s2(units[k - 1], k - 1)
                        yield
                    for h in range(8):
                        P.act(fsq[0:64], onb[0:64, h, :], AF.Square)
                        P.mm(pb[6][0:64, :], cb[0:64, 768:832], fsq[0:64], start=(h == 0), stop=(h == 7))
                    sd = fsd
                    P.act(sd[0:64], pb[6][0:64, :], AF.Ln, bias=cf[0:64, 899:900], scale=1.0 / 512)
                    P.act(sd[0:64], sd[0:64], AF.Exp, scale=-0.5)
                    for h in range(8):
                        P.stt(ybo[0:64, h, :], onb[0:64, h, :], pv[0:64, PVN["fngh0"] + h:PVN["fngh0"] + h + 1], sd[0:64], ALU.mult, ALU.mult)
                    P.dma(STQ, yTd[2:6, :, cs].re("c (hh p) t -> p (c hh) t", hh=2), ybo[0:64])
                    yield

                def driveB(main, side):
                    ms, ss = True, side is not None
                    while ms or ss:
                        if ms:
                            try:
                                next(main)
                            except StopIteration:
                                ms = False
                        if ss:
                            try:
                                next(side)
                            except StopIteration:
                                ss = False

                NBb = NB if 'B' in PASSES else 0
                if NBb:
                    driveB(preB(0, 0), None)
                for blk in range(NBb):
                    driveB(attB(blk, blk % 2), preB(blk + 1, (blk + 1) % 2) if blk + 1 < NBb else None)
                P.end_phase()

            rwkv_pass(P, nc, l, S, consts, banks, pvd, pvc, projT, yTd, lrw)

            moe_pass(P, nc, l, S, consts, banks, pvd, pvc, xsrc, xdst, wr, rb, wg, wu, wd, n2g_d, yTd, w_out)
        print("instructions:", P.ninstr)
    return nc


def rwkv_pass(P, nc, l, S, consts, banks, pvd, pvc, projT, yTd, lrw):
    NB = S // 512
    C1 = 0.6065306597126334
    with P.phase():
        P.cut = int(os.environ['KCUT']) if 'KCUT' in os.environ else None
        cf, cb = consts()
        pP2, pPT2, pZ2 = (P.ps([128, 1024], F32, nm) for nm in ("pP", "pPT", "pZ"))
        pS1 = P.ps([128, 512], F32, "pS")
        pbt = P.ps([128, 1024], BF16, "pbt")
        pb = []
        for w2_ in (pP2, pPT2, pZ2):
            pb.append(T(w2_.ap[:, 0:512], w2_.key + "lo"))
            pb.append(T(w2_.ap[:, 512:1024], w2_.key + "hi"))
        pb.append(pS1)
        pPw, pPTw, pZw = (T(w2_.ap, (w2_.key + "lo", w2_.key + "hi")) for w2_ in (pP2, pPT2, pZ2))
        pv = P.sb([128, NPV], F32, "pv")
        P.dma("sp", pv, pvd[l])
        lrf = P.sb([128, 256], F32, "lrf")
        lrb = P.sb([128, 256], BF16, "lrb")
        P.dma("sp", lrf, lrw[l])
        P.cp("dve", lrb, lrf)
        onesf = P.sb([128, 64], F32, "onesf")
        P.op("pool", lambda e: e.memset(U(onesf), 1.0), w=KEYS(onesf))
        idp = P.sb([128, 64], F32, "idp")
        P.tt("dve", idp, cf[:, 0:64], cf[:, 64:128], ALU.add)
        msk = {}
        for nm, off in (("Ms", 512), ("MTs", 256), ("MTi", 384)):
            m = P.sb([128, 4, 128], F32, "m" + nm)
            for h in range(4):
                P.cp("dve", m[:, h, :], cf[:, off:off + 128])
            msk[nm] = m.re("p h t -> p (h t)")
        pcw = [P.sb([128, 513], F32, f"pcw{i}") for i in range(7)]
        for i in range(7):
            P.op("pool", lambda e, t=pcw[i]: e.memset(U(t[:, 0:1]), 0.0), w=KEYS(pcw[i]))
        xs2 = [[P.sb([128, 512], F32, f"xs{i}") for i in range(7)] for _ in range(2)]
        ew = [P.sb([128, 512], F32, f"ew{i}") for i in range(3)]
        esq = P.sb([128, 512], BF16, "esq")
        etm = P.sb([128, 512], BF16, "etm")
        w_ = [P.sb([128, 512], F32, f"wC{i}") for i in range(8)]
        lrx = P.sb([128, 512], BF16, "lrx")
        sqb = P.sb([128, 512], BF16, "sqb")
        gt2 = [[P.sb([128, 512], F32, f"gt{ct}") for ct in range(2)] for _ in range(2)]
        kmt2 = [[P.sb([128, 512], F32, f"kmt{ct}") for ct in range(2)] for _ in range(2)]
        Rh2 = [[P.sb([128, 512], BF16, f"Rh{ct}") for ct in range(2)] for _ in range(2)]
        Kk2 = [[P.sb([128, 512], BF16, f"Kk{ct}") for ct in range(2)] for _ in range(2)]
        Bh2 = [[P.sb([128, 512], BF16, f"Bh{ct}") for ct in range(2)] for _ in range(2)]
        Kh2 = [[P.sb([128, 512], BF16, f"Kh{ct}") for ct in range(2)] for _ in range(2)]
        tmb = [P.sb([128, 512], BF16, f"tmb{i}") for i in range(3)]
        Vt2 = [[P.sb([128, 4, 128], BF16, f"Vt{ct}") for ct in range(2)] for _ in range(2)]
        Bgt2 = [[P.sb([128, 4, 128], BF16, f"Bgt{ct}") for ct in range(2)] for _ in range(2)]
        Kgt2 = [[P.sb([128, 4, 128], BF16, f"Kgt{ct}") for ct in range(2)] for _ in range(2)]
        eGC2 = [P.sb([128, 2, 8], F32, "eGC") for _ in range(2)]
        oddt2 = [P.sb([64, 2, 4, 512], BF16, "oddt") for _ in range(2)]
        eGCo2 = [P.sb([64, 2, 8], F32, "eGCo") for _ in range(2)]
        cl = {n: P.sb([128, 8, 128], BF16, "cl" + n) for n in
              ("N", "NT", "A3T", "A2T", "A4T", "Y0", "Y1", "Pa", "PTa", "Pb", "PTb", "nTY")}
        QT = P.sb([64, 4, 128], BF16, "QT")
        PhiT = P.sb([64, 4, 64], BF16, "PhiT")
        HLs = P.sb([64, 4, 64], F32, "HLs")
        Hs = [P.sb([64, 4, 64], F32, f"Hs{i}") for i in range(2)]
        Hb = [P.sb([64, 4, 64], BF16, f"Hb{i}") for i in range(2)]
        P.op("pool", lambda e: e.memset(U(Hs[0]), 0.0), w=KEYS(Hs[0]))
        P.op("pool", lambda e: e.memset(U(Hb[0]), 0.0), w=KEYS(Hb[0]))
        oT = P.sb([64, 4, 512], F32, "oT")
        opair = P.sb([128, 2, 512], F32, "opair")
        yco = P.sb([128, 2, 512], BF16, "yco")
        ident_b = cb[:, 0:128]
        bones = cb[:, 640:768]
        nchunk = 0
        def pre(blk, par):
            xs, gt, kmt, Rh, Kk, Bh, Kh = xs2[par], gt2[par], kmt2[par], Rh2[par], Kk2[par], Bh2[par], Kh2[par]
            Vt, Bgt, Kgt, eGC, oddt, eGCo = Vt2[par], Bgt2[par], Kgt2[par], eGC2[par], oddt2[par], eGCo2[par]
            cs = slice(blk * 512, (blk + 1) * 512)
            for i in range(7):
                P.dma("sp", pcw[i][:, 1:513], projT[19 + i, :, cs])
                d = w_[0]
                P.tt("dve", d, pcw[i][:, 0:512], pcw[i][:, 1:513], ALU.subtract)
                P.stt(xs[i], d, pvc(pv, f"mu{i}"), pcw[i][:, 1:513], ALU.mult, ALU.add)
                P.cp("dve", pcw[i][:, 0:1], pcw[i][:, 512:513])
                yield
            P.act(lrx[0:32], xs[6][0:32], AF.Tanh)
            P.cp("act", lrx[32:64], xs[6][32:64])
            P.act(lrx[64:128], xs[6][64:128], AF.Sigmoid)
            for ct in range(2):
                csl = slice(ct * 128, (ct + 1) * 128)
                r_, k_, v_ = xs[ct], xs[2 + ct], xs[4 + ct]
                sg, a_, kx, t1, t2, Gs, eGi, bi = w_
                P.mm(pb[0], lrb[0:32, csl], lrx[0:32])
                P.mm(pb[1], lrb[32:64, csl], lrx[32:64])
                P.mm(pb[2], lrb[64:128, csl], lrx[64:128])
                P.act(sg, pb[0], AF.Sigmoid, bias=pvc(pv, f"w0_{ct}"))
                P.act(a_, pb[1], AF.Sigmoid, bias=pvc(pv, f"a0_{ct}"))
                P.cp("act", gt[ct], pb[2])
                yield
                P.ts("dve", kx, k_, pvc(pv, f"kk_{ct}"), ALU.mult)
                P.act(sqb, kx, AF.Square)
                P.mm(pb[3], bones, sqb)
                P.act(t1, pb[3], AF.Sqrt)
                P.ts("dve", t1, t1, 1e-12, ALU.max)
                P.rcp(t1, t1)
                P.tt("dve", kx, kx, t1, ALU.mult)
                yield
                P.ts("dve", t1, a_, -1.0, ALU.add, pvc(pv, f"ka_{ct}"), ALU.mult)
                P.stt(kmt[ct], t1, 1.0, k_, ALU.add, ALU.mult)
                P.tt("dve", bi, kx, a_, ALU.mult)
                for c in range(8):
                    P.scan(Gs[:, c * 64:(c + 1) * 64], onesf, sg[:, c * 64:(c + 1) * 64], 0.0)
                yield
                P.act(t1, Gs, AF.Exp, scale=-C1)
                P.tt("dve", Rh[ct], r_, t1, ALU.mult)
                P.cp("dve", eGC[:, ct, :], t1.re("p (c j) -> p c j", j=64)[:, :, 63])
                P.tt("dve", t2, Gs, sg, ALU.subtract)
                P.act(t2, t2, AF.Exp, scale=-C1)
                P.tt("dve", Kk[ct], kx, t2, ALU.mult)
                yield
                P.act(eGi, Gs, AF.Exp, scale=C1)
                P.tt("dve", bi, bi, eGi, ALU.mult)
                P.cp("act", Bh[ct], bi)
                P.tt("dve", t2, kmt[ct], eGi, ALU.mult)
                P.cp("act", Kh[ct], t2)
                yield
                egb = eGC[:, ct, :].bc(2, [128, 8, 64])
                P.tt("dve", tmb[0].re("p (c j) -> p c j", j=64), bi.re("p (c j) -> p c j", j=64), egb, ALU.mult)
                P.tt("dve", tmb[1].re("p (c j) -> p c j", j=64), t2.re("p (c j) -> p c j", j=64), egb, ALU.mult)
                P.cp("act", tmb[2], v_)
                for src, dst in ((tmb[0], Bgt[ct]), (tmb[1], Kgt[ct]), (tmb[2], Vt[ct])):
                    for i in range(4):
                        P.tr(pbt[:, i * 128:(i + 1) * 128], src[:, i * 128:(i + 1) * 128], ident_b)
                    P.cp("act", dst.re("p i c -> p (i c)"), pbt[:, 0:512])
                    yield
            for ct in range(2):
                for kd, tl in enumerate((Rh, Kk, Bh, Kh)):
                    P.dma("sp", oddt[0:64, ct, kd, :], tl[ct][64:128, :])
            P.dma("sp", eGCo, eGC[64:128, :, :])
            yield

        def til(blk, par):
            nonlocal nchunk
            xs, gt, kmt, Rh, Kk, Bh, Kh = xs2[par], gt2[par], kmt2[par], Rh2[par], Kk2[par], Bh2[par], Kh2[par]
            Vt, Bgt, Kgt, eGC, oddt, eGCo = Vt2[par], Bgt2[par], Kgt2[par], eGC2[par], oddt2[par], eGCo2[par]
            cs = slice(blk * 512, (blk + 1) * 512)

            def hsl(kd, h, cols):
                ct_ = h // 2
                if h % 2 == 0:
                    return (Rh, Kk, Bh, Kh)[kd][ct_][0:64, cols]
                return oddt[0:64, ct_, kd, cols]

            id64 = ident_b[0:64, 0:64]
            f4 = "p h t -> p (h t)"

            def hs(h):
                return h // 2, (h % 2) * 64

            for ip in range(2):
                for tp in range(2):
                    i = 2 * ip + tp
                    ts_ = slice(i * 128, (i + 1) * 128)
                    s0 = tp * 4
                    for bki in range(5):
                        for h in range(4):
                            hc = slice(h * 128, (h + 1) * 128)
                            kk_s, bh_s = hsl(1, h, ts_), hsl(2, h, ts_)
                            rh_s, kh_s = hsl(0, h, ts_), hsl(3, h, ts_)
                            l_, r_2 = [(kk_s, bh_s), (bh_s, kk_s), (bh_s, rh_s), (kh_s, kk_s), (kh_s, rh_s)][bki]
                            P.mm(pb[bki][:, hc], l_, r_2)
                    P.tt("dve", cl["N"][:, s0:s0 + 4, :].re(f4), pb[0], msk["Ms"], ALU.mult)
                    P.tt("dve", cl["NT"][:, s0:s0 + 4, :].re(f4), pb[1], msk["MTs"], ALU.mult)
                    P.tt("dve", cl["A3T"][:, s0:s0 + 4, :].re(f4), pb[2], msk["MTi"], ALU.mult)
                    P.tt("dve", cl["A2T"][:, s0:s0 + 4, :].re(f4), pb[3], msk["MTs"], ALU.mult)
                    P.tt("dve", cl["A4T"][:, s0:s0 + 4, :].re(f4), pb[4], msk["MTi"], ALU.mult)
                    yield
                    for h in range(4):
                        ct, hb = hs(h)
                        P.mm(pb[5][:, h * 128:h * 128 + 64], cl["A2T"][:, s0 + h, :], Vt[ct][:, i, hb:hb + 64])
                        P.mm(pb[5][:, h * 128 + 64:(h + 1) * 128], hsl(1, h, ts_), id64)
                    P.cp("act", cl["Y0"][:, s0:s0 + 4, :].re(f4), pb[5])
                    yield
                for sl_ in range(8):
                    P.mm(pZw[:, sl_ * 128:(sl_ + 1) * 128], cl["NT"][:, sl_, :], cl["Y0"][:, sl_, :])
                P.tt("dve", cl["Y1"].re(f4), cl["Y0"].re(f4), pZw, ALU.subtract)
                Pm, PT, Yc, Yn = cl["N"], cl["NT"], cl["Y1"], cl["Y0"]
                nxt = [("Pa", "PTa"), ("Pb", "PTb")]
                for it in range(5):
                    Pn, PTn = cl[nxt[it % 2][0]], cl[nxt[it % 2][1]]
                    for sl_ in range(8):
                        hc = slice(sl_ * 128, (sl_ + 1) * 128)
                        if it < 4:
                            P.mm(pPw[:, hc], PT[:, sl_, :], Pm[:, sl_, :])
                        P.mm(pPTw[:, hc], Pm[:, sl_, :], PT[:, sl_, :])
                    if it < 4:
                        P.cp("act", Pn.re(f4), pPw)
                    P.cp("dve", PTn.re(f4), pPTw)
                    for sl_ in range(8):
                        P.mm(pZw[:, sl_ * 128:(sl_ + 1) * 128], PTn[:, sl_, :], Yc[:, sl_, :])
                    if it < 4:
                        P.tt("dve", Yn.re(f4), Yc.re(f4), pZw, ALU.add)
                        Yc, Yn = Yn, Yc
                    else:
                        P.stt(cl["nTY"].re(f4), pZw, -1.0, Yc.re(f4), ALU.mult, ALU.subtract)
                    Pm, PT = Pn, PTn
                    yield
                nTY = cl["nTY"]
                for tp in range(2):
                    i = 2 * ip + tp
                    ts_ = slice(i * 128, (i + 1) * 128)
                    s0 = tp * 4
                    for h in range(4):
                        hc = slice(h * 128, (h + 1) * 128)
                        P.mm(pb[2][0:64, hc], nTY[:, s0 + h, 64:128], cl["A3T"][:, s0 + h, :], start=True, stop=False)
                        P.mm(pb[2][0:64, hc], id64, hsl(0, h, ts_), start=False, stop=True)
                    P.cp("act", QT.re(f4), pb[2][0:64, :])
                    yield
                    for h in range(4):
                        ct, hb = hs(h)
                        hc = slice(h * 128, (h + 1) * 128)
                        P.mm(pb[5][0:64, hc], Vt[ct][:, i, hb:hb + 64], cl["A4T"][:, s0 + h, :], start=(h == 0), stop=False, skip=True)
                        P.mm(pb[5][0:64, hc], nTY[:, s0 + h, 0:64], cl["A3T"][:, s0 + h, :], start=False, stop=False, skip=True)
                    for cc in range(2):
                        rs = slice(cc * 64, (cc + 1) * 64)
                        cidx = i * 2 + cc
                        Hcur, Hnx = Hs[nchunk % 2], Hs[(nchunk + 1) % 2]
                        Hbc, Hbn = Hb[nchunk % 2], Hb[(nchunk + 1) % 2]
                        nchunk += 1
                        for h in range(4):
                            P.mm(pb[5][0:64, h * 128 + cc * 64:h * 128 + cc * 64 + 64], Hbc[:, h, :], QT[:, h, rs],
                                 start=False, stop=(cc == 1), skip=True)
                        for h in range(4):
                            ct, hb = hs(h)
                            hq = slice(h * 64, (h + 1) * 64)
                            P.mm(pb[3][0:64, hq], nTY[rs, s0 + h, 64:128], Bgt[ct][rs, i, hb:hb + 64])
                            P.mm(pb[4][0:64, hq], Bgt[ct][rs, i, hb:hb + 64], nTY[rs, s0 + h, 0:64], start=True, stop=False)
                            P.mm(pb[4][0:64, hq], Kgt[ct][rs, i, hb:hb + 64], Vt[ct][rs, i, hb:hb + 64], start=False, stop=True)
                        for h in range(4):
                            egs = (eGC[0:64, h // 2, cidx:cidx + 1] if h % 2 == 0 else eGCo[0:64, h // 2, cidx:cidx + 1])
                            P.stt(PhiT[:, h, :], cf[0:64, 0:64], egs, pb[3][0:64, h * 64:(h + 1) * 64], ALU.mult, ALU.add)
                        P.cp("act", HLs.re("p h k -> p (h k)"), pb[4][0:64, 0:256])
                        for h in range(4):
                            P.mm(pb[0][0:64, h * 64:(h + 1) * 64], PhiT[:, h, :], Hbc[:, h, :])
                        P.tt("dve", Hnx.re("p h k -> p (h k)"), pb[0][0:64, 0:256], HLs.re("p h k -> p (h k)"), ALU.add)
                        P.cp("act", Hbn.re("p h k -> p (h k)"), Hnx.re("p h k -> p (h k)"))
                        yield
                    P.cp("act", oT[:, :, ts_], pb[5][0:64, :].re("p (h t) -> p h t", h=4))
                    yield
            for h in range(4):
                P.dma("sp", opair[(h % 2) * 64:(h % 2) * 64 + 64, h // 2, :], oT[0:64, h, :])
            for ct in range(2):
                r_, v_ = xs[ct], xs[4 + ct]
                o_ = opair[:, ct, :]
                t1, t2, t3 = ew[0], ew[1], ew[2]
                P.cp("act", esq, o_)
                P.mm(pb[0], bones, esq)
                P.stt(t1, pb[0], -1.0 / 64, o_, ALU.mult, ALU.add)
                P.act(esq, t1, AF.Square)
                P.mm(pb[1], bones, esq)
                P.act(t2, pb[1], AF.Ln, bias=cf[:, 900:901], scale=1.0 / 64)
                P.act(t2, t2, AF.Exp, scale=-0.5)
                P.tt("dve", t1, t1, t2, ALU.mult)
                P.ts("dve", t1, t1, pvc(pv, f"lng_{ct}"), ALU.mult, pvc(pv, f"lnb_{ct}"), ALU.add)
                P.stt(etm, r_, pvc(pv, f"rk_{ct}"), kmt[ct], ALU.mult, ALU.mult)
                P.mm(pb[2], bones, etm)
                P.tt("dve", t3, pb[2], v_, ALU.mult)
                P.tt("dve", t1, t1, t3, ALU.add)
                P.tt("dve", yco[:, ct, :], t1, gt[ct], ALU.mult)
                yield
            P.dma(STQ, yTd[6:8, :, cs].re("c p t -> p c t"), yco)
            yield

        def drive(main, side):
            ms, ss = True, side is not None
            while ms or ss:
                if ms:
                    try:
                        next(main)
                    except StopIteration:
                        ms = False
                if ss:
                    try:
                        next(side)
                    except StopIteration:
                        ss = False

        NBc = NB if 'C' in PASSES else 0
        if NBc:
            drive(pre(0, 0), None)
        for blk in range(NBc):
            drive(til(blk, blk % 2), pre(blk + 1, (blk + 1) % 2) if blk + 1 < NBc else None)
        P.cut = None
        P.end_phase()


def moe_pass(P, nc, l, S, consts, banks, pvd, pvc, xmid, xdst, wr, rb, wg, wu, wd, n2g_d, yTd, w_out):
    HT = min(S, 2048)
    NH = S // HT
    NTT = HT // 128
    with P.phase():
        P.cut = int(os.environ['KCUTM']) if 'KCUTM' in os.environ else None
        cf, cb = consts()
        pb, pbt = banks(7)
        pv = P.sb([128, NPV], F32, "pv")
        P.dma("sp", pv, pvd[l])
        acc_all = P.sb([128, NTT, D], F32, "acc")

        def accT(i):
            return T(acc_all.ap[:, i, :], f"acc_t{i}")
        h2T = P.sb([128, 8, HT], BF16, "h2T")
        h2f = P.sb([128, 8, 128], F32, "h2f")
        hf = P.sb([128, D], F32, "hf")
        wrf = P.sb([128, 8, 20], F32, "wrf")
        P.dma("sp", wrf, wr[l].re("(c p) n -> p c n", p=128))
        g2bc = P.sb([128, D], F32, "g2bc")
        P.dma("sp", g2bc, T(U(n2g_d[l]).partition_broadcast(128), n2g_d.key))
        wrb = P.sb([128, 8, 20], BF16, "wrb")
        P.cp("dve", wrb, wrf)
        hbb = P.sb([128, D], BF16, "hbb")
        rbb = P.sb([128, 20], F32, "rbb")
        P.dma("sp", rbb, T(U(rb[l]).partition_broadcast(128), rb.key))
        comb = P.sb([128, NTT, 16], F32, "comb")
        LG = P.sb([128, NTT, 20], F32, "LG")
        g4 = P.sb([128, NTT, 4], F32, "g4")
        ge4 = P.sb([128, NTT, 4], F32, "ge4")
        ohg4 = P.sb([128, NTT, 4], F32, "ohg4")
        pen4 = P.sb([128, NTT, 4], F32, "pen4")
        E1 = P.sb([128, NTT, 16], F32, "E1")
        E2 = P.sb([128, NTT, 16], F32, "E2")
        OH1 = P.sb([128, NTT, 16], F32, "OH1")
        OH2 = P.sb([128, NTT, 16], F32, "OH2")
        r1, r2, gp_, m1_, m2_, df_, w1_, w2_ = [P.sb([128, 16], F32, f"rt{j}") for j in range(8)]
        sm = P.sb([128, 64], F32, "sm")
        lg = P.sb([128, 20], F32, "lg")
        e1 = P.sb([128, 16], F32, "e1")
        e2 = P.sb([128, 16], F32, "e2")
        oh = P.sb([128, 16], F32, "oh")
        st4 = P.sb([128, 2], F32, "st4")
        wgu = [P.sb([128, 8, 512], BF16, f"wgu{i}") for i in range(2)]
        wdb = [P.sb([128, 2, D], BF16, f"wdb{i}") for i in range(2)]
        sgt2 = [P.sb([128, 256], F32, f"sgt{i}") for i in range(2)]
        hid2 = [P.sb([128, 256], BF16, f"hid{i}") for i in range(2)]
        hidT2 = [P.sb([128, 2, 128], BF16, f"hidT{i}") for i in range(2)]
        wob = P.sb([128, 8, D], BF16, "wob")
        P.dma("pool", wob, w_out[l].re("(c p) n -> p c n", p=128))
        ytb = [P.sb([128, 8, 512], BF16, f"ytb{i}") for i in range(2)]
        hbb2 = [hbb, P.sb([128, D], BF16, "hbb1")]
        st4b = P.sb([128, 4], F32, "st4b")
        for hf_i in range(NH if 'M' in PASSES else 0):
            t0 = hf_i * HT
            for i in range(NTT):
                P.dma("sp", accT(i), xmid[t0 + i * 128:t0 + (i + 1) * 128, :])
            def tok_s1(i):
                yb_ = ytb[(i // 4) % 2]
                if i % 4 == 0:
                    P.dma("sp", yb_, yTd[:, :, t0 + i * 128:t0 + i * 128 + 512].re("c p t -> p c t"))
                for nb in range(2):
                    bk = pb[3 + 2 * (i % 2) + nb]
                    for c in range(8):
                        P.mm(bk, yb_[:, c, (i % 4) * 128:(i % 4 + 1) * 128], wob[:, c, nb * 512:(nb + 1) * 512],
                             start=(c == 0), stop=(c == 7))
                    P.tt("dve", accT(i)[:, nb * 512:(nb + 1) * 512], accT(i)[:, nb * 512:(nb + 1) * 512], bk, ALU.add)
                ss, rs = st4b[:, 2 * (i % 2):2 * (i % 2) + 1], st4b[:, 2 * (i % 2) + 1:2 * (i % 2) + 2]
                P.act(hf, accT(i), AF.Square, accum=ss)
                P.ts("dve", rs, ss, 1.0 / D, ALU.mult, NORM_EPS, ALU.add)
                P.act(rs, rs, AF.Sqrt)
                P.rcp(rs, rs)
                P.stt(hbb2[i % 2], accT(i), rs, g2bc, ALU.mult, ALU.mult)

            def tok_s2(i):
                hb_ = hbb2[i % 2]
                for c in range(8):
                    P.tr(pbt[:, c * 128:(c + 1) * 128], hb_[:, c * 128:(c + 1) * 128], cb[:, 0:128])
                P.cp("act", h2T[:, :, i * 128:(i + 1) * 128], pbt.re("p (c t) -> p c t", c=8))
                for c in range(8):
                    P.mm(pb[2][:, 0:20], h2T[:, c, i * 128:(i + 1) * 128], wrb[:, c, :], start=(c == 0), stop=(c == 7))
                P.tt("dve", LG[:, i, :], pb[2][:, 0:20], rbb, ALU.add)

            for k in range(NTT + 1):
                if k < NTT:
                    tok_s1(k)
                if k >= 1:
                    tok_s2(k - 1)
            N_ = NTT
            gl, el = LG[:, :, 0:4], LG[:, :, 4:20]
            P.red(r1[:, 0:N_], gl, ALU.max)
            P.tt("dve", g4, gl, r1[:, 0:N_].bc(2, [128, N_, 4]), ALU.subtract)
            P.act(ge4, g4, AF.Exp)
            P.red(r2[:, 0:N_], ge4, ALU.add)
            P.rcp(gp_[:, 0:N_], r2[:, 0:N_])
            P.ts("dve", ohg4, g4, 0.0, ALU.is_equal)
            P.ts("dve", pen4, ohg4, -1.0, ALU.add, 1e30, ALU.mult)
            el4 = el.re("p t (g j) -> p t g j", j=4)
            E1_4 = E1.re("p t (g j) -> p t g j", j=4)
            P.tt("dve", E1_4, el4, ohg4.bc(3, [128, N_, 4, 4]), ALU.mult)
            P.tt("dve", E1_4, E1_4, pen4.bc(3, [128, N_, 4, 4]), ALU.add)
            P.red(m1_[:, 0:N_], E1, ALU.max)
            P.tt("dve", OH1, E1, m1_[:, 0:N_].bc(2, [128, N_, 16]), ALU.is_equal)
            P.stt(E2.re("p t e -> p (t e)"), OH1.re("p t e -> p (t e)"), -1e30, E1.re("p t e -> p (t e)"), ALU.mult, ALU.add)
            P.red(m2_[:, 0:N_], E2, ALU.max)
            P.tt("dve", OH2, E2, m2_[:, 0:N_].bc(2, [128, N_, 16]), ALU.is_equal)
            P.tt("dve", df_[:, 0:N_], m1_[:, 0:N_], m2_[:, 0:N_], ALU.subtract)
            P.act(w1_[:, 0:N_], df_[:, 0:N_], AF.Sigmoid)
            P.act(w2_[:, 0:N_], df_[:, 0:N_], AF.Sigmoid, scale=-1.0)
            P.tt("dve", w1_[:, 0:N_], w1_[:, 0:N_], gp_[:, 0:N_], ALU.mult)
            P.tt("dve", w2_[:, 0:N_], w2_[:, 0:N_], gp_[:, 0:N_], ALU.mult)
            P.tt("dve", OH1, OH1, w1_[:, 0:N_].bc(2, [128, N_, 16]), ALU.mult)
            P.tt("dve", OH2, OH2, w2_[:, 0:N_].bc(2, [128, N_, 16]), ALU.mult)
            P.tt("dve", comb, OH1, OH2, ALU.add)

            def load_expert(e):
                bi = e % 2
                P.dma("pool", wgu[bi][:, :, 0:256], wg[l, e].re("(c p) f -> p c f", p=128))
                P.dma("pool", wgu[bi][:, :, 256:512], wu[l, e].re("(c p) f -> p c f", p=128))
                P.dma("pool", wdb[bi], wd[l, e].re("(c p) n -> p c n", p=128))

            munits = [(e, i) for e in range(NE) for i in range(NTT)]

            def m_s1(u, k):
                e, i = u
                if i == 0:
                    load_expert(e)
                bi, pg = e % 2, pb[k % 2]
                for c in range(8):
                    P.mm(pg, h2T[:, c, i * 128:(i + 1) * 128], wgu[bi][:, c, :], start=(c == 0), stop=(c == 7))
                P.act(sgt2[k % 2], pg[:, 0:256], AF.Silu)
                P.stt(hid2[k % 2], pg[:, 256:512], comb[:, i, e:e + 1], sgt2[k % 2], ALU.mult, ALU.mult)

            def m_s2(u, k):
                sl_ = slice((k % 2) * 256, (k % 2) * 256 + 256)
                for f in range(2):
                    P.tr(pbt[:, sl_.start + f * 128:sl_.start + (f + 1) * 128], hid2[k % 2][:, f * 128:(f + 1) * 128], cb[:, 0:128])
                P.cp("act", hidT2[k % 2].re("p f t -> p (f t)"), pbt[:, sl_])

            def m_s3(u, k):
                e, i = u
                bi = e % 2
                for nb in range(2):
                    bk = pb[3 + 2 * (k % 2) + nb]
                    for f in range(2):
                        P.mm(bk, hidT2[k % 2][:, f, :], wdb[bi][:, f, nb * 512:(nb + 1) * 512], start=(f == 0), stop=(f == 1))
                    P.tt("dve", accT(i)[:, nb * 512:(nb + 1) * 512], accT(i)[:, nb * 512:(nb + 1) * 512], bk, ALU.add)

            nu = len(munits)
            for k in range(nu + 2):
                if k < nu:
                    m_s1(munits[k], k)
                if 1 <= k <= nu:
                    m_s2(munits[k - 1], k - 1)
                if k >= 2:
                    m_s3(munits[k - 2], k - 2)
            for i in range(NTT):
                P.dma(STQ, xdst[t0 + i * 128:t0 + (i + 1) * 128, :], accT(i))
        P.cut = None
        P.end_phase()


_CACHE = {}


def prep_inputs(inp, L):
    f = lambda a: np.ascontiguousarray(np.asarray(a, dtype=np.float32))
    inp = {k: f(v) for k, v in inp.items()}
    shared = {
        "w_in": inp["w_in"], "w_out": inp["w_out"], "norm1_g": inp["norm1_g"], "norm2_g": inp["norm2_g"],
        "wflrep": np.stack([make_wflrep(inp, l) for l in range(L)]),
        "pv": np.stack([make_pv(inp, l) for l in range(L)]),
        "cst": make_consts(),
        "lruw": np.stack([make_lruw(inp, l) for l in range(L)]),
        "lrw": np.ascontiguousarray(np.concatenate([inp["rwkv_w2"], inp["rwkv_a2"], inp["rwkv_g2"]], axis=1)),
        "wr": np.ascontiguousarray(np.concatenate([inp["router_gw"], inp["router_ew"]], axis=2)),
        "rb": np.ascontiguousarray(np.concatenate([inp["router_gb"], inp["router_eb"]], axis=1)),
        "exp_w_gate": inp["exp_w_gate"], "exp_w_up": inp["exp_w_up"], "exp_w_down": inp["exp_w_down"],
    }
    return inp, shared


def kernel(**inputs):
    x = np.asarray(inputs["x"], dtype=np.float32)
    B, S, _ = x.shape
    L = inputs["w_in"].shape[0]
    inp, shared = prep_inputs(inputs, L)
    key = (S, L)
    if key not in _CACHE:
        _CACHE[key] = build_program(S, L)
    nc = _CACHE[key]
    n = 8
    in_maps = []
    for c in range(n):
        m = dict(shared)
        m["x"] = np.ascontiguousarray(x[c % B])
        in_maps.append(m)
    res = run_bass_kernel_spmd(nc, in_maps, core_ids=list(range(n)))
    return np.stack([res.results[b]["out"] for b in range(B)], axis=0).astype(np.float32)
```

```python
import numpy as np
from contextlib import ExitStack
import concourse.bass as bass
import concourse.mybir as mybir
from concourse.bass_utils import run_bass_kernel_spmd

F32 = mybir.dt.float32
BF16 = mybir.dt.bfloat16
AF = mybir.ActivationFunctionType
ALU = mybir.AluOpType
AX = mybir.AxisListType
ENGS = ("pe", "act", "dve", "pool", "sp")

D = 1024
NE = 16
DIN = 2952
NORM_EPS = 1e-6
GN_EPS = 64e-5
import os
PASSES = os.environ.get('KPASSES', 'PABCOM')
STQ = os.environ.get('KSTQ', 'sp')


class T:
    def __init__(s, ap, key):
        s.ap = ap
        s.key = key

    def __getitem__(s, idx):
        return T(s.ap[idx], s.key)

    def re(s, pat, **kw):
        return T(s.ap.rearrange(pat, **kw), s.key)

    def bc(s, axis, shape):
        return T(s.ap.unsqueeze(axis).broadcast_to(shape), s.key)


def U(x):
    return x.ap if isinstance(x, T) else x


def KEYS(*xs):
    out = []
    for x in xs:
        if isinstance(x, T):
            if isinstance(x.key, tuple):
                out.extend(x.key)
            else:
                out.append(x.key)
    return out


class _Ins:
    __slots__ = ("eng", "fn", "deps", "dma", "sig", "val", "sem", "idx", "flushed", "tail")


class Prog:
    ND = 24

    def __init__(self, nc, stack):
        self.nc = nc
        self.stack = stack
        self.q = {e: [] for e in ENGS}
        self.lastw = {}
        self.readers = {}
        self.ndma = 0
        self.dma_ins = []
        self.nt = 0
        self.csem = {e: stack.enter_context(nc.semaphore(f"c_{e}")) for e in ENGS}
        self.dsem = [stack.enter_context(nc.semaphore(f"d_{i}")) for i in range(self.ND)]
        self.cnt = {e: 0 for e in ENGS}
        self.seen = {e: {} for e in ENGS}
        self.pending = {e: [] for e in ENGS}
        self.last_flushed = {e: None for e in ENGS}
        self.ph = None
        self.ninstr = 0
        self.cut = None

    def phase(self):
        self.ph = ExitStack()
        return self.ph

    def sb(self, shape, dt=F32, name=None):
        self.nt += 1
        nm = name or f"t{self.nt}"
        t = self.ph.enter_context(self.nc.sbuf_tensor(f"{nm}_{self.nt}", list(shape), dt))
        return T(t[:] if not hasattr(t, "ap") or True else t, nm + str(self.nt))

    def ps(self, shape, dt=F32, name=None):
        self.nt += 1
        nm = name or f"p{self.nt}"
        t = self.ph.enter_context(self.nc.psum_tensor(f"{nm}_{self.nt}", list(shape), dt))
        return T(t[:], nm + str(self.nt))

    def _res(self, d):
        if d.flushed and not d.sig:
            return d.tail
        return d

    def op(self, eng, fn, r=(), w=(), dma=False):
        if self.cut is not None:
            self.cut -= 1
            if self.cut < 0:
                return None
            if 'KLIST' in os.environ:
                import sys as _s
                f = _s._getframe(2)
                print('OP', self.cut, eng, f.f_lineno, f.f_locals.get('h'), f.f_locals.get('ct'))
        ins = _Ins()
        ins.eng, ins.fn, ins.dma, ins.sig = eng, fn, dma, dma
        ins.val, ins.sem, ins.flushed, ins.tail = 0, None, False, None
        deps = list(self.pending[eng])
        self.pending[eng] = []
        for k in r:
            lw = self.lastw.get(k)
            if lw is not None:
                deps.append(lw)
        for k in w:
            lw = self.lastw.get(k)
            if lw is not None:
                deps.append(lw)
            deps.extend(self.readers.get(k, ()))
        if dma:
            ins.idx = self.ndma
            if self.ndma >= self.ND:
                deps.append(self.dma_ins[self.ndma - self.ND])
            self.dma_ins.append(ins)
            self.ndma += 1
        dd, seen = [], set()
        for d in deps:
            d = self._res(d)
            if d is None or d is ins or id(d) in seen:
                continue
            if eng == "pe" and d.eng == "pe" and not d.dma:
                continue
            seen.add(id(d))
            dd.append(d)
            d.sig = True
        ins.deps = dd
        for k in r:
            self.readers.setdefault(k, []).append(ins)
        for k in w:
            self.lastw[k] = ins
            self.readers[k] = []
        self.q[eng].append(ins)
        return ins

    def barrier(self):
        deps = [self.last_flushed[e] for e in ENGS if self.last_flushed[e] is not None]
        deps += self.dma_ins[-self.ND:]
        for e in ENGS:
            self.pending[e] = list(deps)

    def flush(self):
        nc = self.nc
        for e in ENGS:
            if self.q[e]:
                tail = None
                for ins in reversed(self.q[e]):
                    if not ins.dma:
                        tail = ins
                        break
                if tail is not None:
                    tail.sig = True
                for ins in self.q[e]:
                    ins.tail = tail
                    if ins.dma:
                        ins.sem = self.dsem[ins.idx % self.ND]
                        ins.val = 16 * (ins.idx // self.ND + 1)
                    elif ins.sig:
                        self.cnt[e] += 1
                        ins.sem = self.csem[e]
                        ins.val = self.cnt[e]
        with nc.Block() as block:
            def run(ename, eobj):
                seen = self.seen[ename]
                for ins in self.q[ename]:
                    for d in ins.deps:
                        key = id(d.sem)
                        if seen.get(key, 0) < d.val:
                            eobj.wait_ge(d.sem, d.val)
                            seen[key] = d.val
                    i = ins.fn(eobj)
                    self.ninstr += 1
                    if ins.dma:
                        i.then_inc(ins.sem, 16)
                    elif ins.sig:
                        i.then_inc(ins.sem, 1)
                for ins in self.q[ename]:
                    if ins.dma:
                        key = id(ins.sem)
                        if seen.get(key, 0) < ins.val:
                            eobj.wait_ge(ins.sem, ins.val)
                            seen[key] = ins.val

            @block.tensor
            def _(e):
                run("pe", e)

            @block.scalar
            def _(e):
                run("act", e)

            @block.vector
            def _(e):
                run("dve", e)

            @block.gpsimd
            def _(e):
                run("pool", e)

            @block.sync
            def _(e):
                run("sp", e)
        for e in ENGS:
            for ins in self.q[e]:
                ins.flushed = True
            if self.q[e]:
                t = self.q[e][-1].tail
                if t is not None:
                    self.last_flushed[e] = t
            self.q[e] = []

    def end_phase(self):
        print('phase sbuf remaining', self.nc.sbuf_bytes_remaining)
        self.flush()
        self.ph.close()
        self.ph = None
        self.barrier()

    def mm(self, out, lhsT, rhs, start=True, stop=True, skip=False):
        self.op("pe", lambda e: e.matmul(out=U(out), lhsT=U(lhsT), rhs=U(rhs), start=start, stop=stop,
                                         skip_group_check=skip),
                r=KEYS(lhsT, rhs), w=KEYS(out))

    def tr(self, out, in_, ident):
        self.op("pe", lambda e: e.transpose(out=U(out), in_=U(in_), identity=U(ident)),
                r=KEYS(in_, ident), w=KEYS(out))

    def act(self, out, in_, func, bias=None, scale=None, accum=None):
        kw = {}
        if bias is not None:
            kw["bias"] = U(bias)
        if scale is not None:
            kw["scale"] = U(scale)
        if accum is not None:
            kw["accum_out"] = U(accum)
        self.op("act", lambda e: e.activation(out=U(out), in_=U(in_), func=func, **kw),
                r=KEYS(in_, bias, scale), w=KEYS(out, accum))

    def ts(self, eng, out, in0, s1, op0, s2=None, op1=None):
        kw = {}
        if op1 is not None:
            kw["op1"] = op1
        self.op(eng, lambda e: e.tensor_scalar(out=U(out), in0=U(in0), scalar1=U(s1), scalar2=U(s2), op0=op0, **kw),
                r=KEYS(in0, s1, s2), w=KEYS(out))

    def tt(self, eng, out, in0, in1, op):
        self.op(eng, lambda e: e.tensor_tensor(out=U(out), in0=U(in0), in1=U(in1), op=op),
                r=KEYS(in0, in1), w=KEYS(out))

    def stt(self, out, in0, sc, in1, op0, op1):
        self.op("dve", lambda e: e.scalar_tensor_tensor(out=U(out), in0=U(in0), scalar=U(sc), in1=U(in1), op0=op0, op1=op1),
                r=KEYS(in0, sc, in1), w=KEYS(out))

    def cp(self, eng, out, in_):
        if eng == "act":
            self.op("act", lambda e: e.copy(out=U(out), in_=U(in_)), r=KEYS(in_), w=KEYS(out))
        else:
            self.op(eng, lambda e: e.tensor_copy(out=U(out), in_=U(in_)), r=KEYS(in_), w=KEYS(out))

    def rcp(self, out, in_):
        self.op("dve", lambda e: e.reciprocal(out=U(out), in_=U(in_)), r=KEYS(in_), w=KEYS(out))

    def scan(self, out, d0, d1, init):
        self.op("dve", lambda e: e.tensor_tensor_scan(out=U(out), data0=U(d0), data1=U(d1), initial=U(init),
                                                      op0=ALU.mult, op1=ALU.add),
                r=KEYS(d0, d1, init), w=KEYS(out))

    def red(self, out, in_, op):
        self.op("dve", lambda e: e.tensor_reduce(out=U(out), in_=U(in_), axis=AX.X, op=op), r=KEYS(in_), w=KEYS(out))

    def dma(self, eng, out, in_, **kw):
        self.op(eng, lambda e: e.dma_start(out=U(out), in_=U(in_), **kw), r=KEYS(in_), w=KEYS(out), dma=True)


CST = {"ident": 0, "triI": 128, "MTs": 256, "MTi": 384, "Ms": 512, "bones": 640, "ones": 768, "aug": 896}
NCST = 904


def make_consts():
    c = np.zeros((128, NCST), np.float32)
    p = np.arange(128)
    c[:, 0:128] = np.eye(128)
    c[:, 128:256] = (p[None, :] >= p[:, None])
    same = (p[:, None] // 64) == (p[None, :] // 64)
    mts = same & (p[None, :] > p[:, None])
    mti = same & (p[None, :] >= p[:, None])
    c[:, 256:384] = mts
    c[:, 384:512] = mti
    c[:, 512:640] = mts.T
    c[:, 640:768] = same
    c[:, 768:896] = 1.0
    r = p % 32
    c[:, 896] = -1.0 * ((r == 1) | (r == 2))
    c[:, 897] = -1.0 * (r == 2)
    c[:, 898] = -1.0 * (r < 3)
    c[:, 899] = NORM_EPS
    c[:, 900] = GN_EPS
    c[:, 901] = 1.0
    c[:, 902] = 1e-24
    return c


PVN = {}


def _pv_layout():
    names = []
    names += [f"n1g{c}" for c in range(8)] + [f"n2g{c}" for c in range(8)]
    for ct in range(2):
        names += [f"cw{j}_{ct}" for j in range(4)] + [f"cb_{ct}", f"ba_{ct}", f"bx_{ct}", f"lam_{ct}", f"ng_{ct}"]
    names += ["gq", "gk"] + [f"fngh{h}" for h in range(8)] + [f"fb{a}" for a in range(3)]
    names += [f"mu{i}" for i in range(7)]
    for ct in range(2):
        names += [f"w0_{ct}", f"a0_{ct}", f"kk_{ct}", f"ka_{ct}", f"rk_{ct}", f"lng_{ct}", f"lnb_{ct}"]
    for i, n in enumerate(names):
        PVN[n] = i
    return len(names)


NPV = _pv_layout()


def aug_head(a, i):
    h = 3 * a + i
    return h if h < 8 else None


def make_pv(inp, l):
    pv = np.zeros((128, NPV), np.float32)
    p = np.arange(128)
    for c in range(8):
        pv[:, PVN[f"n1g{c}"]] = inp["norm1_g"][l, c * 128:(c + 1) * 128]
        pv[:, PVN[f"n2g{c}"]] = inp["norm2_g"][l, c * 128:(c + 1) * 128]
    for ct in range(2):
        sl = slice(ct * 128, (ct + 1) * 128)
        for j in range(4):
            pv[:, PVN[f"cw{j}_{ct}"]] = inp["conv_w"][l, j, sl]
        pv[:, PVN[f"cb_{ct}"]] = inp["conv_b"][l, sl]
        pv[:, PVN[f"ba_{ct}"]] = inp["lru_ba"][l, sl]
        pv[:, PVN[f"bx_{ct}"]] = inp["lru_bx"][l, sl]
        pv[:, PVN[f"lam_{ct}"]] = inp["lru_lambda"][l, sl]
        pv[:, PVN[f"ng_{ct}"]] = inp["lru_norm_g"][l, sl]
        pv[:, PVN[f"w0_{ct}"]] = inp["rwkv_w0"][l, sl]
        pv[:, PVN[f"a0_{ct}"]] = inp["rwkv_a0"][l, sl]
        pv[:, PVN[f"kk_{ct}"]] = inp["rwkv_kk"][l, sl]
        pv[:, PVN[f"ka_{ct}"]] = inp["rwkv_ka"][l, sl]
        pv[:, PVN[f"rk_{ct}"]] = inp["rwkv_rk"][l].reshape(256)[sl]
        pv[:, PVN[f"lng_{ct}"]] = inp["rwkv_ln_g"][l, sl]
        pv[:, PVN[f"lnb_{ct}"]] = inp["rwkv_ln_b"][l, sl]
    pv[:, PVN["gq"]] = inp["fox_qnorm_g"][l][p % 64]
    pv[:, PVN["gk"]] = inp["fox_knorm_g"][l][p % 64]
    for h in range(8):
        pv[0:64, PVN[f"fngh{h}"]] = inp["fox_norm_g"][l, h * 64:(h + 1) * 64]
    for a in range(3):
        for i in range(3):
            h = aug_head(a, i)
            if h is not None:
                pv[32 * i:32 * i + 32, PVN[f"fb{a}"]] = inp["fox_fb"][l, h]
    for i in range(7):
        pv[:, PVN[f"mu{i}"]] = inp["rwkv_mu"][l, i * 128:(i + 1) * 128]
    return pv


def make_wflrep(inp, l):
    w = np.zeros((1024, 288), np.float32)
    for a in range(3):
        for i in range(3):
            h = aug_head(a, i)
            if h is not None:
                for rr in range(3):
                    w[:, a * 96 + 32 * i + rr] = inp["w_in"][l, :, 2048 + h]
    return w


def make_lruw(inp, l):
    m = np.zeros((2, 2, 128, 128), np.float32)
    for ct in range(2):
        for k, nm in enumerate(("lru_wa", "lru_wx")):
            for hh in range(2):
                m[ct, k, hh * 64:(hh + 1) * 64, hh * 64:(hh + 1) * 64] = inp[nm][l, ct * 2 + hh]
    return m


PT_TILES = [(128 * t, 128) for t in range(16)] + [(2048 + 96 * a, 96) for a in range(3)] + \
           [(2336 + 128 * i, 128) for i in range(7)]
NPT = len(PT_TILES)
WINB = 3232


def build_program(S, L, dbg=False):
    nc = bass.Bass("TRN2", target_bir_lowering=False)
    NB = S // 512
    NT = S // 128

    def din(name, shape, dt=F32):
        return T(nc.dram_tensor(name, list(shape), dt, kind="ExternalInput").ap(), name)

    def dscr(name, shape, dt=F32):
        kind = "ExternalOutput" if dbg else "Internal"
        return T(nc.dram_tensor(name, list(shape), dt, kind=kind).ap(), name)

    x_in = din("x", [S, D])
    w_in = din("w_in", [L, D, DIN])
    wfl = din("wflrep", [L, D, 288])
    w_out = din("w_out", [L, D, D])
    pvd = din("pv", [L, 128, NPV])
    n1g_d = din("norm1_g", [L, D])
    n2g_d = din("norm2_g", [L, D])
    cstd = din("cst", [128, NCST])
    lruw = din("lruw", [L, 2, 2, 128, 128])
    lrw = din("lrw", [L, 128, 256])
    wr = din("wr", [L, D, 20])
    rb = din("rb", [L, 20])
    wg = din("exp_w_gate", [L, NE, D, 256])
    wu = din("exp_w_up", [L, NE, D, 256])
    wd = din("exp_w_down", [L, NE, 256, D])
    out = T(nc.dram_tensor("out", [S, D], F32, kind="ExternalOutput").ap(), "out")
    projT = dscr("projT", [NPT, 128, S])
    yTd = dscr("yTd", [8, 128, S], BF16)
    xmid = dscr("xmid", [S, D])
    xl = dscr("xl", [S, D])

    with ExitStack() as st:
        P = Prog(nc, st)

        def consts():
            cf = P.sb([128, NCST], F32, "cf")
            cb = P.sb([128, 896], BF16, "cb")
            P.dma("sp", cf, cstd)
            P.cp("dve", cb, cf[:, 0:896])
            return cf, cb

        def banks(n=7):
            return [P.ps([128, 512], F32, f"pb{i}") for i in range(n)], P.ps([128, 1024], BF16, "pbt")

        for l in range(L):
            xsrc = x_in if l == 0 else xl
            xdst = out if l == L - 1 else xl

            def pvc(pv, name):
                return pv[:, PVN[name]:PVN[name] + 1]

            with P.phase():
                cf, cb = consts()
                pb, pbt = banks(3)
                pv = P.sb([128, NPV], F32, "pv")
                P.dma("sp", pv, pvd[l])
                winb = P.sb([128, 8, WINB], BF16, "winb")
                w3 = w_in[l].re("(c p) n -> p c n", p=128)
                for c in range(8):
                    P.dma("pool", winb[:, c, 0:2048], w3[:, c, 0:2048])
                P.dma("pool", winb[:, :, 2336:3232], w3[:, :, 2056:2952])
                P.dma("pool", winb[:, :, 2048:2336], wfl[l].re("(c p) n -> p c n", p=128))
                gbc = P.sb([128, D], F32, "gbc")
                P.dma("sp", gbc, T(U(n1g_d[l]).partition_broadcast(128), n1g_d.key))
                xb = P.sb([128, 4, D], F32, "xb")
                junk = P.sb([128, D], F32, "junk")
                hb = P.sb([128, D], BF16, "hb")
                hT2 = [P.sb([128, 8, 512], BF16, "hT") for _ in range(2)]
                st4 = P.sb([128, 8], F32, "st4")
                ost = [P.sb([128, 512], F32, f"ost{i}") for i in range(3)]
                def normP(blk, par):
                    hT = hT2[par]
                    P.dma("sp", xb, xsrc[blk * 512:(blk + 1) * 512, :].re("(i p) d -> p i d", p=128))
                    for i in range(4):
                        ss = st4[:, i:i + 1]
                        rs = st4[:, 4 + i:5 + i]
                        P.act(junk, xb[:, i, :], AF.Square, accum=ss)
                        P.ts("dve", rs, ss, 1.0 / D, ALU.mult, NORM_EPS, ALU.add)
                        P.act(rs, rs, AF.Sqrt)
                        P.rcp(rs, rs)
                        P.stt(hb, xb[:, i, :], rs, gbc, ALU.mult, ALU.mult)
                        for c in range(8):
                            P.tr(pbt[:, c * 128:(c + 1) * 128], hb[:, c * 128:(c + 1) * 128], cb[:, 0:128])
                        P.cp("act", hT[:, :, i * 128:(i + 1) * 128], pbt.re("p (c t) -> p c t", c=8))
                        yield

                def projP(blk, par):
                    hT = hT2[par]
                    for ot, (co, wdt) in enumerate(PT_TILES):
                        bk = pb[ot % 3]
                        for c in range(8):
                            P.mm(bk[0:wdt, :], winb[:, c, co:co + wdt], hT[:, c, :], start=(c == 0), stop=(c == 7))
                        o_ = ost[ot % 3]
                        P.cp("act" if ot % 2 == 0 else "dve", o_[0:wdt, :], bk[0:wdt, :])
                        P.dma(STQ, projT[ot, 0:wdt, blk * 512:(blk + 1) * 512], o_[0:wdt, :])
                        if ot % 6 == 5:
                            yield

                def driveP(main, side):
                    ms, ss = True, side is not None
                    while ms or ss:
                        if ms:
                            try:
                                next(main)
                            except StopIteration:
                                ms = False
                        if ss:
                            try:
                                next(side)
                            except StopIteration:
                                ss = False

                NBp = NB if 'P' in PASSES else 0
                if NBp:
                    driveP(normP(0, 0), None)
                for blk in range(NBp):
                    driveP(projP(blk, blk % 2), normP(blk + 1, (blk + 1) % 2) if blk + 1 < NBp else None)
                P.end_phase()

            with P.phase():
                cf, cb = consts()
                pb, pbt = banks(5)
                pv = P.sb([128, NPV], F32, "pv")
                P.dma("sp", pv, pvd[l])
                lw = P.sb([128, 4, 128], F32, "lw")
                lwb = P.sb([128, 4, 128], BF16, "lwb")
                P.dma("sp", lw, lruw[l].re("a b p n -> p (a b) n"))
                P.cp("dve", lwb, lw)
                cvec = P.sb([128, 2], F32, "cvec")
                for ct in range(2):
                    P.act(cvec[:, ct:ct + 1], pvc(pv, f"lam_{ct}"), AF.Exp, scale=-1.0)
                    P.act(cvec[:, ct:ct + 1], cvec[:, ct:ct + 1], AF.Ln, bias=cf[:, 901:902])
                    P.ts("dve", cvec[:, ct:ct + 1], cvec[:, ct:ct + 1], -8.0, ALU.mult)
                xaw = [P.sb([128, 515], F32, f"xaw{ct}") for ct in range(2)]
                hc = [P.sb([128, 1], F32, f"hc{ct}") for ct in range(2)]
                for ct in range(2):
                    P.op("pool", lambda e, t=xaw[ct]: e.memset(U(t), 0.0), w=KEYS(xaw[ct]))
                    P.op("pool", lambda e, t=hc[ct]: e.memset(U(t), 0.0), w=KEYS(hc[ct]))
                gin = [P.sb([128, 512], F32, f"gin{ct}") for ct in range(2)]
                sA2 = [[P.sb([128, 512], F32, f"sA{ct}_{i}") for i in range(6)] for ct in range(2)]
                xcb2 = [P.sb([128, 512], BF16, f"xcb{ct}") for ct in range(2)]
                yv = [P.sb([128, 512], F32, f"yv{ct}") for ct in range(2)]
                ysq2 = [P.sb([128, 512], BF16, f"ysq{ct}") for ct in range(2)]
                yo = P.sb([128, 2, 512], BF16, "yo")

                def chainA(blk, ct):
                    cs = slice(blk * 512, (blk + 1) * 512)
                    xcb, ysq = xcb2[ct], ysq2[ct]
                    pra, pia = pb[2 * ct], pb[2 * ct + 1]
                    P.dma("sp", xaw[ct][:, 3:515], projT[ct, :, cs])
                    P.dma("sp", gin[ct], projT[2 + ct, :, cs])
                    xw = xaw[ct]
                    xc, r_, i_, a_, t1, t2 = sA2[ct]
                    P.ts("dve", xc, xw[:, 0:512], pvc(pv, f"cw0_{ct}"), ALU.mult, pvc(pv, f"cb_{ct}"), ALU.add)
                    for j in range(1, 4):
                        P.stt(xc, xw[:, j:j + 512], pvc(pv, f"cw{j}_{ct}"), xc, ALU.mult, ALU.add)
                    yield
                    P.cp("dve", t1[:, 0:3], xw[:, 512:515])
                    P.cp("dve", xw[:, 0:3], t1[:, 0:3])
                    P.cp("act", xcb, xc)
                    P.mm(pra, lwb[:, ct * 2 + 0, :], xcb)
                    P.mm(pia, lwb[:, ct * 2 + 1, :], xcb)
                    yield
                    P.act(r_, pra, AF.Sigmoid, bias=pvc(pv, f"ba_{ct}"))
                    P.act(i_, pia, AF.Sigmoid, bias=pvc(pv, f"bx_{ct}"))
                    yield
                    P.act(a_, r_, AF.Exp, scale=cvec[:, ct:ct + 1])
                    P.act(t1, a_, AF.Square)
                    yield
                    P.ts("dve", t1, t1, -1.0, ALU.mult, 1.0, ALU.add)
                    P.act(t1, t1, AF.Sqrt)
                    P.tt("dve", t2, i_, xc, ALU.mult)
                    yield
                    P.tt("dve", t2, t2, t1, ALU.mult)
                    P.scan(r_, a_, t2, hc[ct])
                    P.cp("dve", hc[ct], r_[:, 511:512])
                    yield
                    P.act(i_, gin[ct], AF.Gelu_apprx_tanh)
                    P.tt("dve", yv[ct], i_, r_, ALU.mult)
                    P.act(ysq, yv[ct], AF.Square)
                    yield

                for blk in range(NB if 'A' in PASSES else 0):
                    cs = slice(blk * 512, (blk + 1) * 512)
                    gens = [chainA(blk, 0), chainA(blk, 1)]
                    while gens:
                        for g_ in list(gens):
                            try:
                                next(g_)
                            except StopIteration:
                                gens.remove(g_)
                    for ct in range(2):
                        P.mm(pb[4], cb[:, 768:896], ysq2[ct], start=(ct == 0), stop=(ct == 1))
                    sd = sA2[0][0]
                    P.act(sd, pb[4], AF.Ln, bias=cf[:, 899:900], scale=1.0 / 256)
                    P.act(sd, sd, AF.Exp, scale=-0.5)
                    for ct in range(2):
                        P.stt(yo[:, ct, :], yv[ct], pvc(pv, f"ng_{ct}"), sd, ALU.mult, ALU.mult)
                    P.dma(STQ, yTd[0:2, :, cs].re("c p t -> p c t"), yo)
                P.end_phase()

            with P.phase():
                cf, cb = consts()
                pb, pbt = banks(7)
                pv = P.sb([128, NPV], F32, "pv")
                P.dma("sp", pv, pvd[l])
                gqs = P.sb([128, 1], F32, "gqs")
                P.ts("dve", gqs, pvc(pv, "gq"), 0.125, ALU.mult)
                nfb = P.sb([128, 3], F32, "nfb")
                P.ts("dve", nfb, pv[:, PVN["fb0"]:PVN["fb0"] + 3], -1.0, ALU.mult)
                kT = P.sb([128, 4, S], BF16, "kT")
                Vp = P.sb([128, NT, 8, 65], BF16, "Vp")
                P.op("pool", lambda e: e.memset(U(Vp), 1.0), w=[f"Vp{b}" for b in range(NB)])
                cpT = P.sb([128, NT, 9], F32, "cpT")
                cpc = P.sb([128, 3], F32, "cpc")
                P.op("pool", lambda e: e.memset(U(cpc), 0.0), w=KEYS(cpc))
                qT2 = [P.sb([128, 4, 512], BF16, "qT") for _ in range(2)]
                qaug2 = [P.sb([128, 3, 512], BF16, "qaug") for _ in range(2)]
                fsq = P.sb([128, 512], BF16, "fsq")
                fsd = P.sb([128, 512], F32, "fsd")

                def KB(t, nm, b):
                    return T(t.ap, f"{nm}{b}")
                ld_ = [P.sb([128, 512], F32, f"ldB{i}") for i in range(3)]
                s = [P.sb([128, 512], F32, f"sB{i}") for i in range(5)]
                sqb = P.sb([128, 512], BF16, "sqb")
                vb = P.sb([128, 512], BF16, "vb")
                pex = [P.sb([128, 512], BF16, f"pex{i}") for i in range(6)]
                onb = P.sb([128, 8, 512], F32, "onb")
                rl = P.sb([128, 512], F32, "rl")
                bcs = P.sb([128, 512], F32, "bcs")
                ybo = P.sb([128, 8, 512], BF16, "ybo")
                ones3 = cb[:, 768:896]
                onesf = P.sb([128, 512], F32, "onesf")
                P.op("pool", lambda e: e.memset(U(onesf), 1.0), w=KEYS(onesf))
                npx = 0
                def preB(blk, par):
                    qT, qaug = qT2[par], qaug2[par]
                    cs = slice(blk * 512, (blk + 1) * 512)
                    for which in range(2):
                        for pp in range(4):
                            t_in = ld_[(which * 4 + pp) % 3]
                            P.dma("sp", t_in, projT[4 + which * 4 + pp, :, cs])
                            P.act(sqb, t_in, AF.Square)
                            P.mm(pb[6], cb[:, 640:768], sqb)
                            sd, r1 = s[0], s[1]
                            P.act(sd, pb[6], AF.Ln, bias=cf[:, 899:900], scale=1.0 / 64)
                            P.act(r1, sd, AF.Exp, scale=-0.5)
                            if which == 0:
                                P.stt(qT[:, pp, :], t_in, gqs, r1, ALU.mult, ALU.mult)
                            else:
                                P.stt(KB(kT, "kT", blk)[:, pp, cs], t_in, pvc(pv, "gk"), r1, ALU.mult, ALU.mult)
                            yield
                    for pp in range(4):
                        t_in = ld_[pp % 3]
                        P.dma("sp", t_in, projT[12 + pp, :, cs])
                        P.cp("act", vb, t_in)
                        for i in range(4):
                            P.tr(pbt[:, i * 128:(i + 1) * 128], vb[:, i * 128:(i + 1) * 128], cb[:, 0:128])
                        P.cp("act", KB(Vp, "Vp", blk)[:, blk * 4:blk * 4 + 4, 2 * pp:2 * pp + 2, 0:64],
                             pbt[:, 0:512].re("p (i h d) -> p i h d", i=4, h=2))
                        yield
                    for a in range(3):
                        t_in = ld_[a % 3]
                        P.dma("sp", t_in, projT[16 + a, :, cs])
                        e_, cp_, hi, t1, t2 = s
                        hib = sqb
                        P.act(e_[0:96], t_in[0:96], AF.Exp, bias=nfb[0:96, a:a + 1], scale=-1.0)
                        P.act(e_[0:96], e_[0:96], AF.Ln, bias=cf[0:96, 901:902])
                        P.scan(cp_[0:96], onesf[0:96], e_[0:96], cpc[0:96, a:a + 1])
                        P.cp("dve", cpc[0:96, a:a + 1], cp_[0:96, 511:512])
                        for i in range(4):
                            P.tr(pb[6][:, i * 96:(i + 1) * 96], cp_[0:96, i * 128:(i + 1) * 128], cf[0:96, 0:96])
                        nh = 3 if a < 2 else 2
                        P.cp("act", KB(cpT, "cpT", blk)[:, blk * 4:blk * 4 + 4, 3 * a:3 * a + nh],
                             pb[6][:, 0:384].re("p (i h r) -> p i h r", i=4, h=3)[:, :, 0:nh, 0])
                        P.cp("dve", hib[0:96], cp_[0:96])
                        P.stt(t1[0:96], hib[0:96], cf[0:96, 896:897], cp_[0:96], ALU.mult, ALU.add)
                        P.cp("dve", hib[0:96], t1[0:96])
                        P.stt(t2[0:96], hib[0:96], cf[0:96, 897:898], t1[0:96], ALU.mult, ALU.add)
                        P.ts("dve", qaug[0:96, a, :], t2[0:96], cf[0:96, 898:899], ALU.mult)
                        yield

                def attB(blk, par):
                    qT, qaug = qT2[par], qaug2[par]
                    cs = slice(blk * 512, (blk + 1) * 512)
                    njt = 4 * blk + 4
                    units = [(pp, jt) for pp in range(4) for jt in range(njt)]

                    def att_s1(u, k):
                        pp, jt = u
                        idg = jt - 4 * blk
                        c0 = 0 if idg < 0 else 128 * idg
                        for hh in range(2):
                            h, hb_ = 2 * pp + hh, hh * 64
                            psb = pb[2 + 2 * (k % 2) + hh]
                            P.mm(psb[:, c0:512], KB(kT, "kT", jt // 4)[hb_:hb_ + 64, pp, jt * 128:(jt + 1) * 128], qT[hb_:hb_ + 64, pp, c0:512],
                                 start=True, stop=False)
                        for hh in range(2):
                            h = 2 * pp + hh
                            a, ab = h // 3, 32 * (h % 3)
                            psb = pb[2 + 2 * (k % 2) + hh]
                            P.mm(psb[:, c0:512], ones3[ab:ab + 3, 0:128], qaug[ab:ab + 3, a, c0:512], start=False, stop=True)
                        for hh in range(2):
                            h = 2 * pp + hh
                            psb, px = pb[2 + 2 * (k % 2) + hh], pex[(k % 3) * 2 + hh]
                            P.act(px[:, c0:512], psb[:, c0:512], AF.Exp, bias=KB(cpT, "cpT", jt // 4)[:, jt, h:h + 1])
                            if idg >= 0:
                                P.tt("dve", px[:, c0:c0 + 128], px[:, c0:c0 + 128], cb[:, 128:256], ALU.mult)

                    def att_s2(u, k):
                        pp, jt = u
                        idg = jt - 4 * blk
                        c0 = 0 if idg < 0 else 128 * idg
                        for hh in range(2):
                            h = 2 * pp + hh
                            po, px = pb[hh], pex[(k % 3) * 2 + hh]
                            P.mm(po[0:65, c0:512], KB(Vp, "Vp", jt // 4)[:, jt, h, :], px[:, c0:512], start=(jt == 0), stop=(jt == njt - 1))
                        if jt == njt - 1:
                            for hh in range(2):
                                h = 2 * pp + hh
                                po, pl = pb[hh], pb[6]
                                P.act(rl[64:65, :], po[64:65, :], AF.Ln)
                                P.mm(pl, cf[64:65, 768:896], rl[64:65, :])
                                P.act(bcs[0:64, :], pl[0:64, :], AF.Exp, scale=-1.0)
                                P.tt("dve", onb[0:64, h, :], po[0:64, :], bcs[0:64, :], ALU.mult)

                    for k in range(len(units) + 1):
                        if k < len(units):
                            att_s1(units[k], k)
                        if k >= 1:
                            att_s2(units[k - 1], k - 1)
                        yield
                    for h in range(8):
                        P.act(fsq[0:64], onb[0:64, h, :], AF.Square)
                        P.mm(pb[6][0:64, :], cb[0:64, 768:832], fsq[0:64], start=(h == 0), stop=(h == 7))
                    sd = fsd
                    P.act(sd[0:64], pb[6][0:64, :], AF.Ln, bias=cf[0:64, 899:900], scale=1.0 / 512)
                    P.act(sd[0:64], sd[0:64], AF.Exp, scale=-0.5)
                    for h in range(8):
                        P.stt(ybo[0:64, h, :], onb[0:64, h, :], pv[0:64, PVN["fngh0"] + h:PVN["fngh0"] + h + 1], sd[0:64], ALU.mult, ALU.mult)
                    P.dma(STQ, yTd[2:6, :, cs].re("c (hh p) t -> p (c hh) t", hh=2), ybo[0:64])
                    yield

                def driveB(main, side):
                    ms, ss = True, side is not None
                    while ms or ss:
                        if ms:
                            try:
                                next(main)
                            except StopIteration:
                                ms = False
                        if ss:
                            try:
                                next(side)
                            except StopIteration:
                                ss = False

                NBb = NB if 'B' in PASSES else 0
                if NBb:
                    driveB(preB(0, 0), None)
                for blk in range(NBb):
                    driveB(attB(blk, blk % 2), preB(blk + 1, (blk + 1) % 2) if blk + 1 < NBb else None)
                P.end_phase()

            rwkv_pass(P, nc, l, S, consts, banks, pvd, pvc, projT, yTd, lrw)

            moe_pass(P, nc, l, S, consts, banks, pvd, pvc, xsrc, xdst, wr, rb, wg, wu, wd, n2g_d, yTd, w_out)
        print("instructions:", P.ninstr)
    return nc


def rwkv_pass(P, nc, l, S, consts, banks, pvd, pvc, projT, yTd, lrw):
    NB = S // 512
    C1 = 0.6065306597126334
    with P.phase():
        P.cut = int(os.environ['KCUT']) if 'KCUT' in os.environ else None
        cf, cb = consts()
        pP2, pPT2, pZ2 = (P.ps([128, 1024], F32, nm) for nm in ("pP", "pPT", "pZ"))
        pS1 = P.ps([128, 512], F32, "pS")
        pbt = P.ps([128, 1024], BF16, "pbt")
        pb = []
        for w2_ in (pP2, pPT2, pZ2):
            pb.append(T(w2_.ap[:, 0:512], w2_.key + "lo"))
            pb.append(T(w2_.ap[:, 512:1024], w2_.key + "hi"))
        pb.append(pS1)
        pPw, pPTw, pZw = (T(w2_.ap, (w2_.key + "lo", w2_.key + "hi")) for w2_ in (pP2, pPT2, pZ2))
        pv = P.sb([128, NPV], F32, "pv")
        P.dma("sp", pv, pvd[l])
        lrf = P.sb([128, 256], F32, "lrf")
        lrb = P.sb([128, 256], BF16, "lrb")
        P.dma("sp", lrf, lrw[l])
        P.cp("dve", lrb, lrf)
        onesf = P.sb([128, 64], F32, "onesf")
        P.op("pool", lambda e: e.memset(U(onesf), 1.0), w=KEYS(onesf))
        idp = P.sb([128, 64], F32, "idp")
        P.tt("dve", idp, cf[:, 0:64], cf[:, 64:128], ALU.add)
        msk = {}
        for nm, off in (("Ms", 512), ("MTs", 256), ("MTi", 384)):
            m = P.sb([128, 4, 128], F32, "m" + nm)
            for h in range(4):
                P.cp("dve", m[:, h, :], cf[:, off:off + 128])
            msk[nm] = m.re("p h t -> p (h t)")
        pcw = [P.sb([128, 513], F32, f"pcw{i}") for i in range(7)]
        for i in range(7):
            P.op("pool", lambda e, t=pcw[i]: e.memset(U(t[:, 0:1]), 0.0), w=KEYS(pcw[i]))
        xs2 = [[P.sb([128, 512], F32, f"xs{i}") for i in range(7)] for _ in range(2)]
        ew = [P.sb([128, 512], F32, f"ew{i}") for i in range(3)]
        esq = P.sb([128, 512], BF16, "esq")
        etm = P.sb([128, 512], BF16, "etm")
        w_ = [P.sb([128, 512], F32, f"wC{i}") for i in range(8)]
        lrx = P.sb([128, 512], BF16, "lrx")
        sqb = P.sb([128, 512], BF16, "sqb")
        gt2 = [[P.sb([128, 512], F32, f"gt{ct}") for ct in range(2)] for _ in range(2)]
        kmt2 = [[P.sb([128, 512], F32, f"kmt{ct}") for ct in range(2)] for _ in range(2)]
        Rh2 = [[P.sb([128, 512], BF16, f"Rh{ct}") for ct in range(2)] for _ in range(2)]
        Kk2 = [[P.sb([128, 512], BF16, f"Kk{ct}") for ct in range(2)] for _ in range(2)]
        Bh2 = [[P.sb([128, 512], BF16, f"Bh{ct}") for ct in range(2)] for _ in range(2)]
        Kh2 = [[P.sb([128, 512], BF16, f"Kh{ct}") for ct in range(2)] for _ in range(2)]
        tmb = [P.sb([128, 512], BF16, f"tmb{i}") for i in range(3)]
        Vt2 = [[P.sb([128, 4, 128], BF16, f"Vt{ct}") for ct in range(2)] for _ in range(2)]
        Bgt2 = [[P.sb([128, 4, 128], BF16, f"Bgt{ct}") for ct in range(2)] for _ in range(2)]
        Kgt2 = [[P.sb([128, 4, 128], BF16, f"Kgt{ct}") for ct in range(2)] for _ in range(2)]
        eGC2 = [P.sb([128, 2, 8], F32, "eGC") for _ in range(2)]
        oddt2 = [P.sb([64, 2, 4, 512], BF16, "oddt") for _ in range(2)]
        eGCo2 = [P.sb([64, 2, 8], F32, "eGCo") for _ in range(2)]
        cl = {n: P.sb([128, 8, 128], BF16, "cl" + n) for n in
              ("N", "NT", "A3T", "A2T", "A4T", "Y0", "Y1", "Pa", "PTa", "Pb", "PTb", "nTY")}
        QT = P.sb([64, 4, 128], BF16, "QT")
        PhiT = P.sb([64, 4, 64], BF16, "PhiT")
        HLs = P.sb([64, 4, 64], F32, "HLs")
        Hs = [P.sb([64, 4, 64], F32, f"Hs{i}") for i in range(2)]
        Hb = [P.sb([64, 4, 64], BF16, f"Hb{i}") for i in range(2)]
        P.op("pool", lambda e: e.memset(U(Hs[0]), 0.0), w=KEYS(Hs[0]))
        P.op("pool", lambda e: e.memset(U(Hb[0]), 0.0), w=KEYS(Hb[0]))
        oT = P.sb([64, 4, 512], F32, "oT")
        opair = P.sb([128, 2, 512], F32, "opair")
        yco = P.sb([128, 2, 512], BF16, "yco")
        ident_b = cb[:, 0:128]
        bones = cb[:, 640:768]
        nchunk = 0
        def pre(blk, par):
            xs, gt, kmt, Rh, Kk, Bh, Kh = xs2[par], gt2[par], kmt2[par], Rh2[par], Kk2[par], Bh2[par], Kh2[par]
            Vt, Bgt, Kgt, eGC, oddt, eGCo = Vt2[par], Bgt2[par], Kgt2[par], eGC2[par], oddt2[par], eGCo2[par]
            cs = slice(blk * 512, (blk + 1) * 512)
            for i in range(7):
                P.dma("sp", pcw[i][:, 1:513], projT[19 + i, :, cs])
                d = w_[0]
                P.tt("dve", d, pcw[i][:, 0:512], pcw[i][:, 1:513], ALU.subtract)
                P.stt(xs[i], d, pvc(pv, f"mu{i}"), pcw[i][:, 1:513], ALU.mult, ALU.add)
                P.cp("dve", pcw[i][:, 0:1], pcw[i][:, 512:513])
                yield
            P.act(lrx[0:32], xs[6][0:32], AF.Tanh)
            P.cp("act", lrx[32:64], xs[6][32:64])
            P.act(lrx[64:128], xs[6][64:128], AF.Sigmoid)
            for ct in range(2):
                csl = slice(ct * 128, (ct + 1) * 128)
                r_, k_, v_ = xs[ct], xs[2 + ct], xs[4 + ct]
                sg, a_, kx, t1, t2, Gs, eGi, bi = w_
                P.mm(pb[0], lrb[0:32, csl], lrx[0:32])
                P.mm(pb[1], lrb[32:64, csl], lrx[32:64])
                P.mm(pb[2], lrb[64:128, csl], lrx[64:128])
                P.act(sg, pb[0], AF.Sigmoid, bias=pvc(pv, f"w0_{ct}"))
                P.act(a_, pb[1], AF.Sigmoid, bias=pvc(pv, f"a0_{ct}"))
                P.cp("act", gt[ct], pb[2])
                yield
                P.ts("dve", kx, k_, pvc(pv, f"kk_{ct}"), ALU.mult)
                P.act(sqb, kx, AF.Square)
                P.mm(pb[3], bones, sqb)
                P.act(t1, pb[3], AF.Sqrt)
                P.ts("dve", t1, t1, 1e-12, ALU.max)
                P.rcp(t1, t1)
                P.tt("dve", kx, kx, t1, ALU.mult)
                yield
                P.ts("dve", t1, a_, -1.0, ALU.add, pvc(pv, f"ka_{ct}"), ALU.mult)
                P.stt(kmt[ct], t1, 1.0, k_, ALU.add, ALU.mult)
                P.tt("dve", bi, kx, a_, ALU.mult)
                for c in range(8):
                    P.scan(Gs[:, c * 64:(c + 1) * 64], onesf, sg[:, c * 64:(c + 1) * 64], 0.0)
                yield
                P.act(t1, Gs, AF.Exp, scale=-C1)
                P.tt("dve", Rh[ct], r_, t1, ALU.mult)
                P.cp("dve", eGC[:, ct, :], t1.re("p (c j) -> p c j", j=64)[:, :, 63])
                P.tt("dve", t2, Gs, sg, ALU.subtract)
                P.act(t2, t2, AF.Exp, scale=-C1)
                P.tt("dve", Kk[ct], kx, t2, ALU.mult)
                yield
                P.act(eGi, Gs, AF.Exp, scale=C1)
                P.tt("dve", bi, bi, eGi, ALU.mult)
                P.cp("act", Bh[ct], bi)
                P.tt("dve", t2, kmt[ct], eGi, ALU.mult)
                P.cp("act", Kh[ct], t2)
                yield
                egb = eGC[:, ct, :].bc(2, [128, 8, 64])
                P.tt("dve", tmb[0].re("p (c j) -> p c j", j=64), bi.re("p (c j) -> p c j", j=64), egb, ALU.mult)
                P.tt("dve", tmb[1].re("p (c j) -> p c j", j=64), t2.re("p (c j) -> p c j", j=64), egb, ALU.mult)
                P.cp("act", tmb[2], v_)
                for src, dst in ((tmb[0], Bgt[ct]), (tmb[1], Kgt[ct]), (tmb[2], Vt[ct])):
                    for i in range(4):
                        P.tr(pbt[:, i * 128:(i + 1) * 128], src[:, i * 128:(i + 1) * 128], ident_b)
                    P.cp("act", dst.re("p i c -> p (i c)"), pbt[:, 0:512])
                    yield
            for ct in range(2):
                for kd, tl in enumerate((Rh, Kk, Bh, Kh)):
                    P.dma("sp", oddt[0:64, ct, kd, :], tl[ct][64:128, :])
            P.dma("sp", eGCo, eGC[64:128, :, :])
            yield

        def til(blk, par):
            nonlocal nchunk
            xs, gt, kmt, Rh, Kk, Bh, Kh = xs2[par], gt2[par], kmt2[par], Rh2[par], Kk2[par], Bh2[par], Kh2[par]
            Vt, Bgt, Kgt, eGC, oddt, eGCo = Vt2[par], Bgt2[par], Kgt2[par], eGC2[par], oddt2[par], eGCo2[par]
            cs = slice(blk * 512, (blk + 1) * 512)

            def hsl(kd, h, cols):
                ct_ = h // 2
                if h % 2 == 0:
                    return (Rh, Kk, Bh, Kh)[kd][ct_][0:64, cols]
                return oddt[0:64, ct_, kd, cols]

            id64 = ident_b[0:64, 0:64]
            f4 = "p h t -> p (h t)"

            def hs(h):
                return h // 2, (h % 2) * 64

            for ip in range(2):
                for tp in range(2):
                    i = 2 * ip + tp
                    ts_ = slice(i * 128, (i + 1) * 128)
                    s0 = tp * 4
                    for bki in range(5):
                        for h in range(4):
                            hc = slice(h * 128, (h + 1) * 128)
                            kk_s, bh_s = hsl(1, h, ts_), hsl(2, h, ts_)
                            rh_s, kh_s = hsl(0, h, ts_), hsl(3, h, ts_)
                            l_, r_2 = [(kk_s, bh_s), (bh_s, kk_s), (bh_s, rh_s), (kh_s, kk_s), (kh_s, rh_s)][bki]
                            P.mm(pb[bki][:, hc], l_, r_2)
                    P.tt("dve", cl["N"][:, s0:s0 + 4, :].re(f4), pb[0], msk["Ms"], ALU.mult)
                    P.tt("dve", cl["NT"][:, s0:s0 + 4, :].re(f4), pb[1], msk["MTs"], ALU.mult)
                    P.tt("dve", cl["A3T"][:, s0:s0 + 4, :].re(f4), pb[2], msk["MTi"], ALU.mult)
                    P.tt("dve", cl["A2T"][:, s0:s0 + 4, :].re(f4), pb[3], msk["MTs"], ALU.mult)
                    P.tt("dve", cl["A4T"][:, s0:s0 + 4, :].re(f4), pb[4], msk["MTi"], ALU.mult)
                    yield
                    for h in range(4):
                        ct, hb = hs(h)
                        P.mm(pb[5][:, h * 128:h * 128 + 64], cl["A2T"][:, s0 + h, :], Vt[ct][:, i, hb:hb + 64])
                        P.mm(pb[5][:, h * 128 + 64:(h + 1) * 128], hsl(1, h, ts_), id64)
                    P.cp("act", cl["Y0"][:, s0:s0 + 4, :].re(f4), pb[5])
                    yield
                for sl_ in range(8):
                    P.mm(pZw[:, sl_ * 128:(sl_ + 1) * 128], cl["NT"][:, sl_, :], cl["Y0"][:, sl_, :])
                P.tt("dve", cl["Y1"].re(f4), cl["Y0"].re(f4), pZw, ALU.subtract)
                Pm, PT, Yc, Yn = cl["N"], cl["NT"], cl["Y1"], cl["Y0"]
                nxt = [("Pa", "PTa"), ("Pb", "PTb")]
                for it in range(5):
                    Pn, PTn = cl[nxt[it % 2][0]], cl[nxt[it % 2][1]]
                    for sl_ in range(8):
                        hc = slice(sl_ * 128, (sl_ + 1) * 128)
                        if it < 4:
                            P.mm(pPw[:, hc], PT[:, sl_, :], Pm[:, sl_, :])
                        P.mm(pPTw[:, hc], Pm[:, sl_, :], PT[:, sl_, :])
                    if it < 4:
                        P.cp("act", Pn.re(f4), pPw)
                    P.cp("dve", PTn.re(f4), pPTw)
                    for sl_ in range(8):
                        P.mm(pZw[:, sl_ * 128:(sl_ + 1) * 128], PTn[:, sl_, :], Yc[:, sl_, :])
                    if it < 4:
                        P.tt("dve", Yn.re(f4), Yc.re(f4), pZw, ALU.add)
                        Yc, Yn = Yn, Yc
                    else:
                        P.stt(cl["nTY"].re(f4), pZw, -1.0, Yc.re(f4), ALU.mult, ALU.subtract)
                    Pm, PT = Pn, PTn
                    yield
                nTY = cl["nTY"]
                for tp in range(2):
                    i = 2 * ip + tp
                    ts_ = slice(i * 128, (i + 1) * 128)
                    s0 = tp * 4
                    for h in range(4):
                        hc = slice(h * 128, (h + 1) * 128)
                        P.mm(pb[2][0:64, hc], nTY[:, s0 + h, 64:128], cl["A3T"][:, s0 + h, :], start=True, stop=False)
                        P.mm(pb[2][0:64, hc], id64, hsl(0, h, ts_), start=False, stop=True)
                    P.cp("act", QT.re(f4), pb[2][0:64, :])
                    yield
                    for h in range(4):
                        ct, hb = hs(h)
                        hc = slice(h * 128, (h + 1) * 128)
                        P.mm(pb[5][0:64, hc], Vt[ct][:, i, hb:hb + 64], cl["A4T"][:, s0 + h, :], start=(h == 0), stop=False, skip=True)
                        P.mm(pb[5][0:64, hc], nTY[:, s0 + h, 0:64], cl["A3T"][:, s0 + h, :], start=False, stop=False, skip=True)
                    for cc in range(2):
                        rs = slice(cc * 64, (cc + 1) * 64)
                        cidx = i * 2 + cc
                        Hcur, Hnx = Hs[nchunk % 2], Hs[(nchunk + 1) % 2]
                        Hbc, Hbn = Hb[nchunk % 2], Hb[(nchunk + 1) % 2]
                        nchunk += 1
                        for h in range(4):
                            P.mm(pb[5][0:64, h * 128 + cc * 64:h * 128 + cc * 64 + 64], Hbc[:, h, :], QT[:, h, rs],
                                 start=False, stop=(cc == 1), skip=True)
                        for h in range(4):
                            ct, hb = hs(h)
                            hq = slice(h * 64, (h + 1) * 64)
                            P.mm(pb[3][0:64, hq], nTY[rs, s0 + h, 64:128], Bgt[ct][rs, i, hb:hb + 64])
                            P.mm(pb[4][0:64, hq], Bgt[ct][rs, i, hb:hb + 64], nTY[rs, s0 + h, 0:64], start=(h == 0), stop=False, skip=True)
                            P.mm(pb[4][0:64, hq], Kgt[ct][rs, i, hb:hb + 64], Vt[ct][rs, i, hb:hb + 64], start=False, stop=False, skip=True)
                        for h in range(4):
                            egs = (eGC[0:64, h // 2, cidx:cidx + 1] if h % 2 == 0 else eGCo[0:64, h // 2, cidx:cidx + 1])
                            P.stt(PhiT[:, h, :], cf[0:64, 0:64], egs, pb[3][0:64, h * 64:(h + 1) * 64], ALU.mult, ALU.add)
                        for h in range(4):
                            P.mm(pb[4][0:64, h * 64:(h + 1) * 64], PhiT[:, h, :], Hbc[:, h, :], start=False, stop=(h == 3), skip=True)
                        P.cp("act", Hbn.re("p h k -> p (h k)"), pb[4][0:64, 0:256])
                        yield
                    P.cp("act", oT[:, :, ts_], pb[5][0:64, :].re("p (h t) -> p h t", h=4))
                    yield
            for h in range(4):
                P.dma("sp", opair[(h % 2) * 64:(h % 2) * 64 + 64, h // 2, :], oT[0:64, h, :])
            for ct in range(2):
                r_, v_ = xs[ct], xs[4 + ct]
                o_ = opair[:, ct, :]
                t1, t2, t3 = ew[0], ew[1], ew[2]
                P.cp("act", esq, o_)
                P.mm(pb[0], bones, esq)
                P.stt(t1, pb[0], -1.0 / 64, o_, ALU.mult, ALU.add)
                P.act(esq, t1, AF.Square)
                P.mm(pb[1], bones, esq)
                P.act(t2, pb[1], AF.Ln, bias=cf[:, 900:901], scale=1.0 / 64)
                P.act(t2, t2, AF.Exp, scale=-0.5)
                P.tt("dve", t1, t1, t2, ALU.mult)
                P.ts("dve", t1, t1, pvc(pv, f"lng_{ct}"), ALU.mult, pvc(pv, f"lnb_{ct}"), ALU.add)
                P.stt(etm, r_, pvc(pv, f"rk_{ct}"), kmt[ct], ALU.mult, ALU.mult)
                P.mm(pb[2], bones, etm)
                P.tt("dve", t3, pb[2], v_, ALU.mult)
                P.tt("dve", t1, t1, t3, ALU.add)
                P.tt("dve", yco[:, ct, :], t1, gt[ct], ALU.mult)
                yield
            P.dma(STQ, yTd[6:8, :, cs].re("c p t -> p c t"), yco)
            yield

        def drive(main, side):
            ms, ss = True, side is not None
            while ms or ss:
                if ms:
                    try:
                        next(main)
                    except StopIteration:
                        ms = False
                if ss:
                    try:
                        next(side)
                    except StopIteration:
                        ss = False

        NBc = NB if 'C' in PASSES else 0
        if NBc:
            drive(pre(0, 0), None)
        for blk in range(NBc):
            drive(til(blk, blk % 2), pre(blk + 1, (blk + 1) % 2) if blk + 1 < NBc else None)
        P.cut = None
        P.end_phase()


def moe_pass(P, nc, l, S, consts, banks, pvd, pvc, xmid, xdst, wr, rb, wg, wu, wd, n2g_d, yTd, w_out):
    HT = min(S, 2048)
    NH = S // HT
    NTT = HT // 128
    with P.phase():
        P.cut = int(os.environ['KCUTM']) if 'KCUTM' in os.environ else None
        cf, cb = consts()
        pb, pbt = banks(7)
        pv = P.sb([128, NPV], F32, "pv")
        P.dma("sp", pv, pvd[l])
        acc_all = P.sb([128, NTT, D], F32, "acc")

        def accT(i):
            return T(acc_all.ap[:, i, :], f"acc_t{i}")
        h2T = P.sb([128, 8, HT], BF16, "h2T")
        h2f = P.sb([128, 8, 128], F32, "h2f")
        hf = P.sb([128, D], F32, "hf")
        wrf = P.sb([128, 8, 20], F32, "wrf")
        P.dma("sp", wrf, wr[l].re("(c p) n -> p c n", p=128))
        g2bc = P.sb([128, D], F32, "g2bc")
        P.dma("sp", g2bc, T(U(n2g_d[l]).partition_broadcast(128), n2g_d.key))
        wrb = P.sb([128, 8, 20], BF16, "wrb")
        P.cp("dve", wrb, wrf)
        hbb = P.sb([128, D], BF16, "hbb")
        rbb = P.sb([128, 20], F32, "rbb")
        P.dma("sp", rbb, T(U(rb[l]).partition_broadcast(128), rb.key))
        comb = P.sb([128, NTT, 16], F32, "comb")
        LG = P.sb([128, NTT, 20], F32, "LG")
        g4 = P.sb([128, NTT, 4], F32, "g4")
        ge4 = P.sb([128, NTT, 4], F32, "ge4")
        ohg4 = P.sb([128, NTT, 4], F32, "ohg4")
        pen4 = P.sb([128, NTT, 4], F32, "pen4")
        E1 = P.sb([128, NTT, 16], F32, "E1")
        E2 = P.sb([128, NTT, 16], F32, "E2")
        OH1 = P.sb([128, NTT, 16], F32, "OH1")
        OH2 = P.sb([128, NTT, 16], F32, "OH2")
        r1, r2, gp_, m1_, m2_, df_, w1_, w2_ = [P.sb([128, 16], F32, f"rt{j}") for j in range(8)]
        sm = P.sb([128, 64], F32, "sm")
        lg = P.sb([128, 20], F32, "lg")
        e1 = P.sb([128, 16], F32, "e1")
        e2 = P.sb([128, 16], F32, "e2")
        oh = P.sb([128, 16], F32, "oh")
        st4 = P.sb([128, 2], F32, "st4")
        wgu = [P.sb([128, 8, 512], BF16, f"wgu{i}") for i in range(2)]
        wdb = [P.sb([128, 2, D], BF16, f"wdb{i}") for i in range(2)]
        sgt2 = [P.sb([128, 256], F32, f"sgt{i}") for i in range(2)]
        hid2 = [P.sb([128, 256], BF16, f"hid{i}") for i in range(2)]
        hidT2 = [P.sb([128, 2, 128], BF16, f"hidT{i}") for i in range(2)]
        wob = P.sb([128, 8, D], BF16, "wob")
        P.dma("pool", wob, w_out[l].re("(c p) n -> p c n", p=128))
        ytb = [P.sb([128, 8, 512], BF16, f"ytb{i}") for i in range(2)]
        hbb2 = [hbb, P.sb([128, D], BF16, "hbb1")]
        st4b = P.sb([128, 4], F32, "st4b")
        for hf_i in range(NH if 'M' in PASSES else 0):
            t0 = hf_i * HT
            for i in range(NTT):
                P.dma("sp", accT(i), xmid[t0 + i * 128:t0 + (i + 1) * 128, :])
            def tok_s1(i):
                yb_ = ytb[(i // 4) % 2]
                if i % 4 == 0:
                    P.dma("sp", yb_, yTd[:, :, t0 + i * 128:t0 + i * 128 + 512].re("c p t -> p c t"))
                for nb in range(2):
                    bk = pb[3 + 2 * (i % 2) + nb]
                    for c in range(8):
                        P.mm(bk, yb_[:, c, (i % 4) * 128:(i % 4 + 1) * 128], wob[:, c, nb * 512:(nb + 1) * 512],
                             start=(c == 0), stop=(c == 7))
                    P.tt("dve", accT(i)[:, nb * 512:(nb + 1) * 512], accT(i)[:, nb * 512:(nb + 1) * 512], bk, ALU.add)
                ss, rs = st4b[:, 2 * (i % 2):2 * (i % 2) + 1], st4b[:, 2 * (i % 2) + 1:2 * (i % 2) + 2]
                P.act(hf, accT(i), AF.Square, accum=ss)
                P.ts("dve", rs, ss, 1.0 / D, ALU.mult, NORM_EPS, ALU.add)
                P.act(rs, rs, AF.Sqrt)
                P.rcp(rs, rs)
                P.stt(hbb2[i % 2], accT(i), rs, g2bc, ALU.mult, ALU.mult)

            def tok_s2(i):
                hb_ = hbb2[i % 2]
                for c in range(8):
                    P.tr(pbt[:, c * 128:(c + 1) * 128], hb_[:, c * 128:(c + 1) * 128], cb[:, 0:128])
                P.cp("act", h2T[:, :, i * 128:(i + 1) * 128], pbt.re("p (c t) -> p c t", c=8))
                for c in range(8):
                    P.mm(pb[2][:, 0:20], h2T[:, c, i * 128:(i + 1) * 128], wrb[:, c, :], start=(c == 0), stop=(c == 7))
                P.tt("dve", LG[:, i, :], pb[2][:, 0:20], rbb, ALU.add)

            for k in range(NTT + 1):
                if k < NTT:
                    tok_s1(k)
                if k >= 1:
                    tok_s2(k - 1)
            N_ = NTT
            gl, el = LG[:, :, 0:4], LG[:, :, 4:20]
            P.red(r1[:, 0:N_], gl, ALU.max)
            P.tt("dve", g4, gl, r1[:, 0:N_].bc(2, [128, N_, 4]), ALU.subtract)
            P.act(ge4, g4, AF.Exp)
            P.red(r2[:, 0:N_], ge4, ALU.add)
            P.rcp(gp_[:, 0:N_], r2[:, 0:N_])
            P.ts("dve", ohg4, g4, 0.0, ALU.is_equal)
            P.ts("dve", pen4, ohg4, -1.0, ALU.add, 1e30, ALU.mult)
            el4 = el.re("p t (g j) -> p t g j", j=4)
            E1_4 = E1.re("p t (g j) -> p t g j", j=4)
            P.tt("dve", E1_4, el4, ohg4.bc(3, [128, N_, 4, 4]), ALU.mult)
            P.tt("dve", E1_4, E1_4, pen4.bc(3, [128, N_, 4, 4]), ALU.add)
            P.red(m1_[:, 0:N_], E1, ALU.max)
            P.tt("dve", OH1, E1, m1_[:, 0:N_].bc(2, [128, N_, 16]), ALU.is_equal)
            P.stt(E2.re("p t e -> p (t e)"), OH1.re("p t e -> p (t e)"), -1e30, E1.re("p t e -> p (t e)"), ALU.mult, ALU.add)
            P.red(m2_[:, 0:N_], E2, ALU.max)
            P.tt("dve", OH2, E2, m2_[:, 0:N_].bc(2, [128, N_, 16]), ALU.is_equal)
            P.tt("dve", df_[:, 0:N_], m1_[:, 0:N_], m2_[:, 0:N_], ALU.subtract)
            P.act(w1_[:, 0:N_], df_[:, 0:N_], AF.Sigmoid)
            P.act(w2_[:, 0:N_], df_[:, 0:N_], AF.Sigmoid, scale=-1.0)
            P.tt("dve", w1_[:, 0:N_], w1_[:, 0:N_], gp_[:, 0:N_], ALU.mult)
            P.tt("dve", w2_[:, 0:N_], w2_[:, 0:N_], gp_[:, 0:N_], ALU.mult)
            P.tt("dve", OH1, OH1, w1_[:, 0:N_].bc(2, [128, N_, 16]), ALU.mult)
            P.tt("dve", OH2, OH2, w2_[:, 0:N_].bc(2, [128, N_, 16]), ALU.mult)
            P.tt("dve", comb, OH1, OH2, ALU.add)

            def load_expert(e):
                bi = e % 2
                P.dma("pool", wgu[bi][:, :, 0:256], wg[l, e].re("(c p) f -> p c f", p=128))
                P.dma("pool", wgu[bi][:, :, 256:512], wu[l, e].re("(c p) f -> p c f", p=128))
                P.dma("pool", wdb[bi], wd[l, e].re("(c p) n -> p c n", p=128))

            munits = [(e, i) for e in range(NE) for i in range(NTT)]

            def m_s1(u, k):
                e, i = u
                if i == 0:
                    load_expert(e)
                bi, pg = e % 2, pb[k % 2]
                for c in range(8):
                    P.mm(pg, h2T[:, c, i * 128:(i + 1) * 128], wgu[bi][:, c, :], start=(c == 0), stop=(c == 7))
                P.act(sgt2[k % 2], pg[:, 0:256], AF.Silu)
                P.stt(hid2[k % 2], pg[:, 256:512], comb[:, i, e:e + 1], sgt2[k % 2], ALU.mult, ALU.mult)

            def m_s2(u, k):
                sl_ = slice((k % 2) * 256, (k % 2) * 256 + 256)
                for f in range(2):
                    P.tr(pbt[:, sl_.start + f * 128:sl_.start + (f + 1) * 128], hid2[k % 2][:, f * 128:(f + 1) * 128], cb[:, 0:128])
                P.cp("act", hidT2[k % 2].re("p f t -> p (f t)"), pbt[:, sl_])

            def m_s3(u, k):
                e, i = u
                bi = e % 2
                for nb in range(2):
                    bk = pb[3 + 2 * (k % 2) + nb]
                    for f in range(2):
                        P.mm(bk, hidT2[k % 2][:, f, :], wdb[bi][:, f, nb * 512:(nb + 1) * 512], start=(f == 0), stop=(f == 1))
                    P.tt("dve", accT(i)[:, nb * 512:(nb + 1) * 512], accT(i)[:, nb * 512:(nb + 1) * 512], bk, ALU.add)

            nu = len(munits)
            for k in range(nu + 2):
                if k < nu:
                    m_s1(munits[k], k)
                if 1 <= k <= nu:
                    m_s2(munits[k - 1], k - 1)
                if k >= 2:
                    m_s3(munits[k - 2], k - 2)
            for i in range(NTT):
                P.dma(STQ, xdst[t0 + i * 128:t0 + (i + 1) * 128, :], accT(i))
        P.cut = None
        P.end_phase()


_CACHE = {}


def prep_inputs(inp, L):
    f = lambda a: np.ascontiguousarray(np.asarray(a, dtype=np.float32))
    inp = {k: f(v) for k, v in inp.items()}
    shared = {
        "w_in": inp["w_in"], "w_out": inp["w_out"], "norm1_g": inp["norm1_g"], "norm2_g": inp["norm2_g"],
        "wflrep": np.stack([make_wflrep(inp, l) for l in range(L)]),
        "pv": np.stack([make_pv(inp, l) for l in range(L)]),
        "cst": make_consts(),
        "lruw": np.stack([make_lruw(inp, l) for l in range(L)]),
        "lrw": np.ascontiguousarray(np.concatenate([inp["rwkv_w2"], inp["rwkv_a2"], inp["rwkv_g2"]], axis=1)),
        "wr": np.ascontiguousarray(np.concatenate([inp["router_gw"], inp["router_ew"]], axis=2)),
        "rb": np.ascontiguousarray(np.concatenate([inp["router_gb"], inp["router_eb"]], axis=1)),
        "exp_w_gate": inp["exp_w_gate"], "exp_w_up": inp["exp_w_up"], "exp_w_down": inp["exp_w_down"],
    }
    return inp, shared


def kernel(**inputs):
    x = np.asarray(inputs["x"], dtype=np.float32)
    B, S, _ = x.shape
    L = inputs["w_in"].shape[0]
    inp, shared = prep_inputs(inputs, L)
    key = (S, L)
    if key not in _CACHE:
        _CACHE[key] = build_program(S, L)
    nc = _CACHE[key]
    n = 8
    in_maps = []
    for c in range(n):
        m = dict(shared)
        m["x"] = np.ascontiguousarray(x[c % B])
        in_maps.append(m)
    res = run_bass_kernel_spmd(nc, in_maps, core_ids=list(range(n)))
    return np.stack([res.results[b]["out"] for b in range(B)], axis=0).astype(np.float32)
```
